# Optimizing a Trainium2 kernel written in Bass

```python
import jax
import jax.numpy as jnp
from jax import lax
import numpy as np

D_MODEL = 1024
BATCH = 8
SEQ = 2048
DEPTH = 1

GRID_W = 64
HEAD_DIM = 64
NA_HEADS = 8
NA_WIN_H = 8
NA_WIN_W = 16
GQA_HEADS = 8
GQA_KV_HEADS = 2
Q_BLOCK = 128
ROPE_THETA = 10000.0
PEER_HEADS = 8
PEER_NKEYS = 128
PEER_EXPERTS = PEER_NKEYS * PEER_NKEYS
PEER_QDIM = 256
PEER_HALF = PEER_QDIM // 2
PEER_TOPK = 16
PEER_CHUNK = 128
EPS = 1e-6

NA_WIDTH = NA_HEADS * HEAD_DIM
GQA_Q_WIDTH = GQA_HEADS * HEAD_DIM
GQA_KV_WIDTH = GQA_KV_HEADS * HEAD_DIM
IN_SPLITS = (NA_WIDTH, NA_WIDTH, NA_WIDTH, GQA_Q_WIDTH, GQA_KV_WIDTH, GQA_KV_WIDTH, D_MODEL, D_MODEL)
IN_WIDTH = sum(IN_SPLITS)
IN_SPLIT_POINTS = tuple(int(p) for p in np.cumsum(IN_SPLITS)[:-1])

kernel_name = "hybrid_natten_gqa_peer_encoder"


def rmsnorm(x, g):
    xf = x.astype(jnp.float32)
    y = xf * lax.rsqrt(jnp.mean(xf * xf, axis=-1, keepdims=True) + EPS)
    return (y * g.astype(jnp.float32)).astype(x.dtype)


def neighbourhood_attention(q, k, v, rpb):
    b, s, h, dh = q.shape
    rows = s // GRID_W
    kh = min(NA_WIN_H, rows)
    q = q.reshape(b, rows, GRID_W, h, dh)
    k = k.reshape(b, rows, GRID_W, h, dh)
    v = v.reshape(b, rows, GRID_W, h, dh)
    r = jnp.arange(rows)
    row_start = jnp.clip(r - kh // 2, 0, rows - kh)
    key_rows = row_start[:, None] + jnp.arange(kh)[None, :]
    k_g = k[:, key_rows]
    v_g = v[:, key_rows]
    c = jnp.arange(GRID_W)
    col_start = jnp.clip(c - NA_WIN_W // 2, 0, GRID_W - NA_WIN_W)
    in_win = (c[None, :] >= col_start[:, None]) & (c[None, :] < col_start[:, None] + NA_WIN_W)
    dr = key_rows - r[:, None] + (NA_WIN_H - 1)
    dc = jnp.clip(c[None, :] - c[:, None], -(NA_WIN_W - 1), NA_WIN_W - 1) + (NA_WIN_W - 1)
    bias = rpb[:, dr[:, None, :, None], dc[None, :, None, :]].astype(jnp.float32)
    scores = jnp.einsum("brqhd,brkwhd->bhrqkw", q, k_g).astype(jnp.float32) * (dh ** -0.5) + bias[None]
    scores = jnp.where(in_win[:, None, :], scores, -jnp.inf)
    probs = jax.nn.softmax(scores.reshape(b, h, rows, GRID_W, kh * GRID_W), axis=-1)
    probs = probs.reshape(b, h, rows, GRID_W, kh, GRID_W).astype(v.dtype)
    out = jnp.einsum("bhrqkw,brkwhd->brqhd", probs, v_g)
    return out.reshape(b, s, h * dh)


def _rotate(x, ang):
    half = x.shape[-1] // 2
    x1, x2 = x[..., :half], x[..., half:]
    cos = jnp.cos(ang)[None, :, None, :]
    sin = jnp.sin(ang)[None, :, None, :]
    return jnp.concatenate([x1 * cos - x2 * sin, x2 * cos + x1 * sin], axis=-1)


def axial_rope(x):
    b, s, h, dh = x.shape
    sec = dh // 2
    nf = sec // 2
    t = jnp.arange(s)
    inv = ROPE_THETA ** (-jnp.arange(nf, dtype=jnp.float32) / nf)
    ang_r = (t // GRID_W).astype(jnp.float32)[:, None] * inv[None, :]
    ang_c = (t % GRID_W).astype(jnp.float32)[:, None] * inv[None, :]
    xf = x.astype(jnp.float32)
    out = jnp.concatenate([_rotate(xf[..., :sec], ang_r), _rotate(xf[..., sec:], ang_c)], axis=-1)
    return out.astype(x.dtype)


def gqa_attention(q, k, v):
    b, s, h, dh = q.shape
    hkv = k.shape[2]
    grp = h // hkv
    nblk = s // Q_BLOCK
    qb = q.reshape(b, nblk, Q_BLOCK, hkv, grp, dh).transpose(1, 0, 2, 3, 4, 5)
    scale = dh ** -0.5

    def block(qblk):
        sc = jnp.einsum("bqngd,bknd->bngqk", qblk, k).astype(jnp.float32) * scale
        p = jax.nn.softmax(sc, axis=-1).astype(v.dtype)
        return jnp.einsum("bngqk,bknd->bqngd", p, v)

    out = lax.map(block, qb)
    return out.transpose(1, 0, 2, 3, 4, 5).reshape(b, s, h * dh)


def peer(xn, w_q, sub_keys, u, v):
    b, s, d = xn.shape
    t = b * s
    xt = xn.reshape(t, d)
    q = (xt @ w_q).reshape(t, PEER_HEADS, 2, PEER_HALF)
    sc = jnp.einsum("thpc,hpnc->thpn", q, sub_keys).astype(jnp.float32)
    top_s, top_i = lax.top_k(sc, PEER_TOPK)
    cand_s = (top_s[:, :, 0, :, None] + top_s[:, :, 1, None, :]).reshape(t, PEER_HEADS, PEER_TOPK * PEER_TOPK)
    cand_i = (top_i[:, :, 0, :, None] * PEER_NKEYS + top_i[:, :, 1, None, :]).reshape(t, PEER_HEADS, PEER_TOPK * PEER_TOPK)
    best_s, best_pos = lax.top_k(cand_s, PEER_TOPK)
    idx = jnp.take_along_axis(cand_i, best_pos, axis=-1)
    gate = jax.nn.softmax(best_s, axis=-1)
    nchunk = t // PEER_CHUNK
    x_c = xt.reshape(nchunk, PEER_CHUNK, d)
    i_c = idx.reshape(nchunk, PEER_CHUNK, PEER_HEADS * PEER_TOPK)
    g_c = gate.reshape(nchunk, PEER_CHUNK, PEER_HEADS * PEER_TOPK)

    def chunk(args):
        xc, ic, gc = args
        uc = u[ic]
        act = jnp.einsum("cd,ced->ce", xc, uc).astype(jnp.float32)
        hid = (jax.nn.gelu(act, approximate=False) * gc).astype(xc.dtype)
        return jnp.einsum("ce,ced->cd", hid, v[ic])

    out = lax.map(chunk, (x_c, i_c, g_c))
    return out.reshape(b, s, d)


def setup_inputs(seed: int = 0) -> dict:
    key = jax.random.key(seed)
    ks = jax.random.split(key, 16)
    f32 = jnp.float32
    nrm = lambda k, shape, scale: jax.random.normal(k, shape, f32) * scale
    return {
        "x": nrm(ks[0], (BATCH, SEQ, D_MODEL), 1.0),
        "norm1_g": 1.0 + nrm(ks[1], (DEPTH, D_MODEL), 0.1),
        "w_in": nrm(ks[2], (DEPTH, D_MODEL, IN_WIDTH), D_MODEL ** -0.5),
        "na_rpb": nrm(ks[3], (DEPTH, NA_HEADS, 2 * NA_WIN_H - 1, 2 * NA_WIN_W - 1), 0.5),
        "gqa_q_norm_g": 1.0 + nrm(ks[4], (DEPTH, HEAD_DIM), 0.1),
        "gqa_k_norm_g": 1.0 + nrm(ks[5], (DEPTH, HEAD_DIM), 0.1),
        "w_proj_a": nrm(ks[6], (DEPTH, NA_WIDTH, D_MODEL), NA_WIDTH ** -0.5),
        "w_proj_b": nrm(ks[7], (DEPTH, GQA_Q_WIDTH, D_MODEL), GQA_Q_WIDTH ** -0.5),
        "w_out": nrm(ks[8], (DEPTH, D_MODEL, D_MODEL), D_MODEL ** -0.5),
        "norm2_g": 1.0 + nrm(ks[9], (DEPTH, D_MODEL), 0.1),
        "peer_w_q": nrm(ks[10], (DEPTH, D_MODEL, PEER_HEADS * PEER_QDIM), D_MODEL ** -0.5),
        "peer_sub_keys": nrm(ks[11], (DEPTH, PEER_HEADS, 2, PEER_NKEYS, PEER_HALF), PEER_HALF ** -0.5),
        "peer_u": nrm(ks[12], (DEPTH, PEER_EXPERTS, D_MODEL), D_MODEL ** -0.5),
        "peer_v": nrm(ks[13], (DEPTH, PEER_EXPERTS, D_MODEL), PEER_HEADS ** -0.5),
        "norm_f_g": 1.0 + nrm(ks[14], (D_MODEL,), 0.1),
    }


def reference(x, norm1_g, w_in, na_rpb, gqa_q_norm_g, gqa_k_norm_g, w_proj_a, w_proj_b, w_out,
              norm2_g, peer_w_q, peer_sub_keys, peer_u, peer_v, norm_f_g):
    b, s, _ = x.shape
    for l in range(DEPTH):
        h = rmsnorm(x, norm1_g[l])
        proj = h @ w_in[l]
        qa, ka, va, qb, kb, vb, ga, gb = jnp.split(proj, IN_SPLIT_POINTS, axis=-1)
        ya = neighbourhood_attention(
            qa.reshape(b, s, NA_HEADS, HEAD_DIM),
            ka.reshape(b, s, NA_HEADS, HEAD_DIM),
            va.reshape(b, s, NA_HEADS, HEAD_DIM),
            na_rpb[l])
        qh = axial_rope(rmsnorm(qb.reshape(b, s, GQA_HEADS, HEAD_DIM), gqa_q_norm_g[l]))
        kh = axial_rope(rmsnorm(kb.reshape(b, s, GQA_KV_HEADS, HEAD_DIM), gqa_k_norm_g[l]))
        yb = gqa_attention(qh, kh, vb.reshape(b, s, GQA_KV_HEADS, HEAD_DIM))
        merged = jax.nn.sigmoid(ga) * (ya @ w_proj_a[l]) + jax.nn.sigmoid(gb) * (yb @ w_proj_b[l])
        x = x + merged @ w_out[l]
        x = x + peer(rmsnorm(x, norm2_g[l]), peer_w_q[l], peer_sub_keys[l], peer_u[l], peer_v[l])
    return rmsnorm(x, norm_f_g)
```

```python
import contextlib
import numpy as np
import ml_dtypes
import concourse.bass as bass
import concourse.mybir as mybir
from concourse.bass_utils import run_bass_kernel_spmd

F32 = mybir.dt.float32
BF16 = mybir.dt.bfloat16
U32 = mybir.dt.uint32
ALU = mybir.AluOpType
AF = mybir.ActivationFunctionType

S = 2048
D = 1024
NT = 16
EPS = 1e-6
NEG = -30000.0


class Buf:
    __slots__ = ("name", "last_w", "readers", "writers")

    def __init__(self, name):
        self.name = name
        self.last_w = None
        self.readers = []
        self.writers = []


class Prog:
    ENGS = ("pe", "dve", "act", "pool", "sp")

    def __init__(self, nc, es):
        self.nc = nc
        self.es = es
        self.ops = []
        self.sem = {}
        self.cnt = {}
        self.opsig = []
        self.last_eng = {}
        self.last_dma = {}
        self.pending = {}
        self.sigflag = []

    def _sem(self, key):
        if key not in self.sem:
            self.sem[key] = self.es.enter_context(self.nc.semaphore("s%d" % len(self.sem)))
            self.cnt[key] = 0
        return self.sem[key]

    def _compact(self, lst):
        if len(lst) <= 48:
            return lst
        best = {}
        keep = []
        for o in lst:
            sg = self.opsig[o]
            if sg is None:
                keep.append(o)
            elif sg[0] not in best or self.opsig[best[sg[0]]][1] < sg[1]:
                best[sg[0]] = o
        return keep + list(best.values())

    def op(self, eng, fn, reads=(), writes=(), dma=False, semkey=None, extra_deps=(), sig=True, wdis=()):
        deps = set(extra_deps)
        for b in reads:
            if b.last_w is not None:
                deps.add(b.last_w)
            deps.update(b.writers)
        for b in writes:
            if b.last_w is not None:
                deps.add(b.last_w)
            deps.update(b.writers)
            deps.update(b.readers)
        for b in wdis:
            if b.last_w is not None:
                deps.add(b.last_w)
            deps.update(b.readers)
        oid = len(self.ops)
        if dma:
            key = ("dma", semkey)
            inc = 16
            self.last_dma[key] = oid
        else:
            key = ("eng", eng)
            inc = 1
            self.last_eng[eng] = oid
        self._sem(key)
        if sig:
            self.cnt[key] += inc
            self.opsig.append((key, self.cnt[key]))
            for po in self.pending.pop(key, []):
                self.opsig[po] = (key, self.cnt[key])
        else:
            assert not dma
            self.opsig.append(None)
            self.pending.setdefault(key, []).append(oid)
        self.sigflag.append(sig)
        self.ops.append((eng, fn, sorted(deps), dma))
        for b in reads:
            b.readers.append(oid)
            b.readers = self._compact(b.readers)
        for b in writes:
            b.last_w = oid
            b.readers = []
            b.writers = []
        for b in wdis:
            b.writers.append(oid)
            b.writers = self._compact(b.writers)
        return oid

    def barrier(self):
        deps = list(self.last_eng.values()) + list(self.last_dma.values())
        for eng in self.ENGS:
            self.op(eng, None, extra_deps=deps)

    def emit(self, final_wait_ops=()):
        nc = self.nc
        assert not any(self.pending.values()), "unsignalled trailing ops"
        block = self.es.enter_context(nc.Block())
        per_eng = {e: [] for e in self.ENGS}
        for oid, (eng, fn, deps, dma) in enumerate(self.ops):
            per_eng[eng].append(oid)
        prog = self

        def make(engname):
            def body(e):
                waited = {}
                for oid in per_eng[engname]:
                    eng, fn, deps, dma = prog.ops[oid]
                    need = {}
                    for d in deps:
                        deng, _, _, ddma = prog.ops[d]
                        if (not ddma) and deng == engname and engname == "pe":
                            continue
                        k, v = prog.opsig[d]
                        if need.get(k, 0) < v:
                            need[k] = v
                    for k, v in need.items():
                        if waited.get(k, 0) < v:
                            e.wait_ge(prog.sem[k], v)
                            waited[k] = v
                    k, v = prog.opsig[oid]
                    if fn is None:
                        e.sem_inc(prog.sem[k], 1)
                    else:
                        ins = fn(e)
                        if prog.sigflag[oid]:
                            ins.then_inc(prog.sem[k], 16 if dma else 1)
                if engname == "sp":
                    need = {}
                    for d in final_wait_ops:
                        k, v = prog.opsig[d]
                        if need.get(k, 0) < v:
                            need[k] = v
                    for k, v in need.items():
                        e.wait_ge(prog.sem[k], v)
            return body

        block.tensor(make("pe"))
        block.vector(make("dve"))
        block.scalar(make("act"))
        block.gpsimd(make("pool"))
        block.sync(make("sp"))


def _dtsize(dt):
    return 2 if dt == BF16 else 4


class Arena:
    def __init__(self, ar, nbytes):
        self.ar = ar
        self.nbytes = nbytes
        self.top = 0

    def alloc(self, shape, dt):
        n = int(np.prod(shape))
        nb = n * _dtsize(dt)
        off = self.top
        self.top += (nb + 63) // 64 * 64
        assert self.top <= self.nbytes, ("arena overflow", self.top, self.nbytes)
        v = self.ar[:, off // 2:(off + nb) // 2]
        if dt != BF16:
            v = v.bitcast(dt)
        if len(shape) == 2:
            v = v.rearrange("p (a b) -> p a b", a=shape[0])
        elif len(shape) == 3:
            v = v.rearrange("p (a b c) -> p a b c", a=shape[0], b=shape[1])
        elif len(shape) == 4:
            v = v.rearrange("p (a b c d) -> p a b c d", a=shape[0], b=shape[1], c=shape[2])
        return v


def cap(view, rel, dims, parts=None, pstart=0):
    ps = view.ap[0][0]
    npart = parts if parts is not None else view.ap[0][1]
    return bass.AP(tensor=view.tensor, offset=view.offset + pstart * ps + rel,
                   ap=[[ps, npart]] + [list(d) for d in dims])


def dap(t, off, dims):
    return bass.AP(tensor=t, offset=off, ap=[list(d) for d in dims])


ARENA_BYTES = 207 * 1024


NA_SUB = 9
NA_ROWS = 32
PK_TILES = 1


def build(stage=99, dbg=()):
    nc = bass.Bass("TRN2", target_bir_lowering=False)
    es = contextlib.ExitStack()

    def din(name, shape, dt=F32):
        return nc.dram_tensor(name, list(shape), dt, kind="ExternalInput")

    x_d = din("x", [S, D])
    g1_d = din("norm1_g", [1, D])
    win_d = din("w_in", [D, 4352])
    rb_d = din("rbg", [128, 8 * 14 * 64])
    nm_d = din("negmask", [128, 64])
    gq_d = din("gq", [128, 1])
    gk_d = din("gk", [128, 1])
    wpa_d = din("w_proj_a", [512, D])
    wpb_d = din("w_proj_b", [512, D])
    wo_d = din("w_out", [D, D])
    g2_d = din("norm2_g", [1, D])
    wq_d = din("peer_w_q", [D, 2048])
    sk_d = din("peer_sub_keys", [16 * 128, 128])
    u_d = din("peer_u", [16384, D])
    v_d = din("peer_v", [16384, D])
    gf_d = din("norm_f_g", [1, D])
    idb_d = din("identb", [128, 128], BF16)
    idf_d = din("identf", [128, 128])
    iob_d = din("iotab", [128, 128], BF16)
    io16_d = din("iota16", [128, 16])
    ropec_d = din("rope_c", [128, S])
    ropes_d = din("rope_s", [128, S])
    perm_d = din("permT", [128, 128])
    blk_d = din("blk64", [128, 128])
    out_d = nc.dram_tensor("out", [S, D], F32, kind="ExternalOutput")
    x1s_d = nc.dram_tensor("x1s", [S, D], F32, kind="Internal")
    vs_d = nc.dram_tensor("vs", [16384, D], BF16, kind="Internal")
    uts_d = nc.dram_tensor("uts", [128, 128 * 1024], BF16, kind="Internal")
    dbg_d = {}
    for name, shape in dbg:
        dbg_d[name] = nc.dram_tensor("dbg_" + name, list(shape), F32, kind="ExternalOutput")

    ar = es.enter_context(nc.sbuf_tensor("arena", [128, ARENA_BYTES // 2], BF16))
    ps = es.enter_context(nc.psum_tensor("ps", [128, 4096], F32))
    A = Arena(ar, ARENA_BYTES)
    psA = ps[:, :]
    P = Prog(nc, es)
    PB = [Buf("bank%d" % i) for i in range(8)]

    def bank(i, a=0, b=512):
        return ps[:, i * 512 + a:i * 512 + b]

    def bankb(i):
        return ps[:, i * 512:(i + 1) * 512].bitcast(BF16)

    _dq = [0]

    def dma_q():
        _dq[0] += 1
        return "sp"

    identb = A.alloc([128], BF16)
    identf = A.alloc([128], F32)
    iotab = A.alloc([128], BF16)
    iota16 = A.alloc([16], F32)
    B_C = Buf("consts")

    def ld(dst, src, q="sp", key=None):
        P.op(q, lambda e: e.dma_start(out=dst, in_=src), writes=[B_C], dma=True, semkey=("c", key or id(dst)))

    ld(identb, idb_d.ap(), key=0)
    ld(identf, idf_d.ap(), key=1)
    ld(iotab, iob_d.ap(), key=2)
    ld(iota16, io16_d.ap(), key=3)
    stats = A.alloc([64], F32)
    B_ST = [Buf("st%d" % i) for i in range(NT)]
    base_mark = A.top

    hT = A.alloc([8, S], BF16)
    B_HT = [Buf("hT%d" % i) for i in range(4)]
    wbf_mark = A.top
    wbf = [A.alloc([8, 512], BF16) for _ in range(2)]
    B_WB = [Buf("wb0"), Buf("wb1")]
    att_mark = A.top

    def dump(name, view_fn, reads, rows, cols):
        pass

    g1b = A.alloc([D], F32)
    ld(g1b, dap(g1_d, 0, [[0, 128], [1, D]]), key=4)
    xt = [A.alloc([D], F32) for _ in range(2)]
    hb = [A.alloc([D], BF16) for _ in range(2)]
    junk_ref = [A.alloc([D], BF16)]
    B_XT = [Buf("xt0"), Buf("xt1")]
    B_HB = [Buf("hb0"), Buf("hb1")]
    B_JK = Buf("junk")
    ss, rt, rstd = stats[:, 0:16], stats[:, 16:32], stats[:, 32:48]

    def rms_tile(i, src_view, gvec, s, reads_src, B_hb_s, hb_s):
        jk = junk_ref[0]
        P.op("act", lambda e: e.activation(out=jk, in_=src_view, func=AF.Square, accum_out=ss[:, i:i + 1]),
             reads=reads_src, writes=[B_JK, B_ST[i]])
        P.op("act", lambda e: e.activation(out=rt[:, i:i + 1], in_=ss[:, i:i + 1], func=AF.Sqrt, scale=1.0 / D, bias=EPS),
             reads=[B_ST[i]], writes=[B_ST[i]])
        P.op("dve", lambda e: e.reciprocal(out=rstd[:, i:i + 1], in_=rt[:, i:i + 1]), reads=[B_ST[i]], writes=[B_ST[i]])
        P.op("dve", lambda e: e.scalar_tensor_tensor(out=hb_s, in0=src_view, scalar=rstd[:, i:i + 1], in1=gvec,
                                                     op0=ALU.mult, op1=ALU.mult),
             reads=reads_src + [B_ST[i], B_C], writes=[B_hb_s])

    def transpose_tile_to(i, hb_s, B_hb_s, dstT, B_dst, bk):
        pb = bankb(bk)
        for k in range(8):
            P.op("pe", lambda e, k=k: e.transpose(pb[:, k * 128:(k + 1) * 128], hb_s[:, k * 128:(k + 1) * 128], identb),
                 reads=[B_hb_s, B_C], writes=[PB[bk]], sig=(k == 7))
        P.op("act", lambda e: e.activation(out=dstT[:, :, i * 128:(i + 1) * 128],
                                           in_=pb.rearrange("p (k t) -> p k t", k=8), func=AF.Copy),
             reads=[PB[bk]], writes=[B_dst])

    for i in range(NT):
        s = i % 2
        P.op("sp", lambda e, i=i, s=s: e.dma_start(out=xt[s], in_=x_d.ap()[i * 128:(i + 1) * 128, :]),
             writes=[B_XT[s]], dma=True, semkey=("xt", s))
        rms_tile(i, xt[s], g1b, s, [B_XT[s]], B_HB[s], hb[s])
        transpose_tile_to(i, hb[s], B_HB[s], hT, B_HT[i // 4], 6 + s)

    finals = []
    B_DBG = Buf("dbg")

    def dbg_out(name, sb_view, reads, r0, nrows, c0, ncols):
        if name in dbg_d:
            o = P.op("sp", lambda e: e.dma_start(out=dbg_d[name].ap()[r0:r0 + nrows, c0:c0 + ncols], in_=sb_view),
                     reads=reads, writes=[B_DBG], dma=True, semkey=("dbg",))
            finals.append(o)

    if "hT" in dbg_d:
        tmpf = A.alloc([S], F32)
        B_T = Buf("tmpf")
        for k in range(8):
            P.op("dve", lambda e, k=k: e.tensor_copy(out=tmpf, in_=hT[:, k, :]), reads=B_HT, writes=[B_T])
            dbg_out("hT", tmpf, [B_T], k * 128, 128, 0, S)

    if stage <= 0:
        P.emit(finals)
        return nc, es

    P.barrier()
    A.top = att_mark
    yaT = A.alloc([4, S], BF16)
    B_YAT, B_YBT = Buf("yaT"), Buf("ybT")
    if dbg_d:
        P.op("pool", lambda e: e.memset(yaT.rearrange("p a b -> p (a b)"), 0.0), writes=[B_YAT])
    ph_mark = A.top
    QaT = A.alloc([4, S], BF16)
    KaT = A.alloc([8, S], BF16)
    Va = A.alloc([16, 8, 65], BF16)
    Vas = A.alloc([15, 8, 65], BF16)
    RB2 = A.alloc([14, 8, 64], F32)
    negm = A.alloc([64], F32)
    B_QA, B_KA, B_VA, B_VAS, B_RB = Buf("QaT"), Buf("KaT"), Buf("Va"), Buf("Vas"), Buf("RB2")

    P.op("sp", lambda e: e.dma_start(out=RB2.rearrange("p a b c -> p (a b c)"), in_=rb_d.ap()), writes=[B_RB], dma=True, semkey=("c", 10))
    P.op("sp", lambda e: e.dma_start(out=negm, in_=nm_d.ap()), writes=[B_RB], dma=True, semkey=("c", 11))
    P.op("pool", lambda e: e.tensor_tensor(out=RB2.rearrange("p a b c -> p (a b) c"), in0=RB2.rearrange("p a b c -> p (a b) c"),
                                           in1=cap(negm, 0, [[0, 112], [1, 64]]), op=ALU.add), reads=[B_RB], writes=[B_RB])
    P.op("pool", lambda e: e.memset(KaT.rearrange("p a b -> p (a b)"), 0.0), writes=[B_KA])
    P.op("pool", lambda e: e.memset(Va.rearrange("p a b c -> p (a b c)"), 1.0), writes=[B_VA])
    P.op("pool", lambda e: e.memset(Vas.rearrange("p a b c -> p (a b c)"), 1.0), writes=[B_VAS])

    def load_w(slot, src_t, row_stride, c0, ncols, kch=8, dcol=0):
        dst = wbf[slot][:, 0:kch, dcol:dcol + ncols]
        P.op("pool", lambda e: e.dma_start(out=dst, in_=dap(src_t, c0, [[row_stride, 128], [128 * row_stride, kch], [1, ncols]])),
             writes=[B_WB[slot]], dma=True, semkey=("wb", slot))

    _bk = [0]

    def next_bank(n=6):
        b = _bk[0] % n
        _bk[0] += 1
        return b

    _ev = [0]

    def evac(out_view, in_view, reads, writes, eng=None):
        _ev[0] += 1
        if eng == "act" or (eng is None and _ev[0] % 2 == 0):
            P.op("act", lambda e: e.activation(out=out_view, in_=in_view, func=AF.Copy), reads=reads, wdis=writes)
        else:
            P.op("dve", lambda e: e.tensor_copy(out=out_view, in_=in_view), reads=reads, wdis=writes)

    def proj_fm(slot, ncols, dstT, B_dst, post=None):
        for c in range(ncols // 128):
            for tc in range(4):
                b = next_bank()
                for k in range(8):
                    P.op("pe", lambda e, k=k, c=c, tc=tc, b=b: e.matmul(bank(b), lhsT=wbf[slot][:, k, c * 128:(c + 1) * 128],
                                                                        rhs=hT[:, k, tc * 512:(tc + 1) * 512], start=(k == 0), stop=(k == 7)),
                         reads=[B_WB[slot], B_HT[tc]], writes=[PB[b]], sig=(k == 7))
                if post is None:
                    evac(dstT[:, c, tc * 512:(tc + 1) * 512], bank(b), [PB[b]], [B_dst])
                else:
                    post(c, tc, b)

    def proj_tm(slot, ncols, tok_off, ntiles, dst_fn, B_dst):
        for i in range(ntiles):
            b = next_bank()
            t0 = tok_off + i * 128
            for k in range(8):
                P.op("pe", lambda e, k=k, b=b, t0=t0: e.matmul(bank(b, 0, ncols), lhsT=hT[:, k, t0:t0 + 128],
                                                               rhs=wbf[slot][:, k, 0:ncols], start=(k == 0), stop=(k == 7)),
                     reads=[B_WB[slot]] + B_HT, writes=[PB[b]], sig=(k == 7))
            evac(dst_fn(i), bank(b, 0, ncols).rearrange("p (h d) -> p h d", d=64), [PB[b]], [B_dst])

    load_w(0, win_d, 4352, 0, 512)
    load_w(1, win_d, 4352, 512, 512)
    proj_fm(0, 512, QaT, B_QA)
    load_w(0, win_d, 4352, 1024, 512)
    def ka_post(c, tc, b):
        evac(KaT[0:64, 2 * c, tc * 512:(tc + 1) * 512], ps[0:64, b * 512:(b + 1) * 512], [PB[b]], [B_KA])
        evac(KaT[64:128, 2 * c + 1, tc * 512:(tc + 1) * 512], ps[64:128, b * 512:(b + 1) * 512], [PB[b]], [B_KA])

    proj_fm(1, 512, KaT, B_KA, post=ka_post)
    proj_tm(0, 512, 0, 16, lambda i: Va[:, i, :, 0:64], B_VA)
    proj_tm(0, 512, 64, 15, lambda i: Vas[:, i, :, 0:64], B_VAS)
    load_w(1, win_d, 4352, 1536, 512)

    if stage <= 0.5:
        tmpf = A.alloc([S], F32)
        B_T = Buf("tmpf2")
        if "QaT" in dbg_d:
            for k in range(4):
                P.op("dve", lambda e, k=k: e.tensor_copy(out=tmpf, in_=QaT[:, k, :]), reads=[B_QA], writes=[B_T])
                dbg_out("QaT", tmpf, [B_T], k * 128, 128, 0, S)
        if "Va" in dbg_d:
            for i in range(16):
                P.op("dve", lambda e, i=i: e.tensor_copy(out=tmpf[:, 0:512].rearrange("p (h d) -> p h d", d=64), in_=Va[:, i, :, 0:64]), reads=[B_VA], writes=[B_T])
                dbg_out("Va", tmpf[:, 0:512], [B_T], i * 128, 128, 0, 512)
        P.emit(finals)
        return nc, es
    ssb = [A.alloc([1024], F32) for _ in range(2)]
    PT = [A.alloc([1024], BF16) for _ in range(2)]
    yar = [A.alloc([512], BF16) for _ in range(2)]
    rec = A.alloc([2, 4], F32)
    B_SSB = [Buf("ssb0"), Buf("ssb1")]
    B_PT = [Buf("pt0"), Buf("pt1")]
    B_YAR = [Buf("yar0"), Buf("yar1")]
    B_REC = [Buf("rec0"), Buf("rec1")]
    na_steps = [(r, g) for r in range(32) for g in range(2)]

    def na_A(step):
        r, g = na_steps[step]
        rs = min(max(r - 4, 0), 24)
        ks = rs * 64
        dr0 = rs - r + 7
        sl = step % 2
        sb0 = sl * 2
        for hh in range(4):
            h = g * 4 + hh
            for kt in range(4):
                o0 = sb0 * 512 + kt * 256 + hh * 64
                bkw = sb0 + (kt // 2)
                P.op("pe", lambda e, o0=o0, h=h, kt=kt, ks=ks, r=r: e.matmul(
                    ps[:, o0:o0 + 64], lhsT=KaT[:, h, ks + kt * 128:ks + (kt + 1) * 128],
                    rhs=QaT[:, h // 2, r * 64:(r + 1) * 64], start=True, stop=True),
                    reads=[B_KA, B_QA], writes=[PB[bkw]], sig=(hh == 3 and kt == 3))
        P.op("dve", lambda e, sl=sl, sb0=sb0, g=g, dr0=dr0: e.scalar_tensor_tensor(
            out=ssb[sl].rearrange("p (k q) -> p k q", k=4),
            in0=ps[:, sb0 * 512:sb0 * 512 + 1024].rearrange("p (k q) -> p k q", k=4),
            scalar=0.125, in1=cap(RB2, (dr0 * 8 + g * 4) * 64, [[1024, 4], [1, 256]]),
            op0=ALU.mult, op1=ALU.add),
            reads=[PB[sb0], PB[sb0 + 1], B_RB], writes=[B_SSB[sl]])
        P.op("act", lambda e, sl=sl: e.activation(out=PT[sl], in_=ssb[sl], func=AF.Exp), reads=[B_SSB[sl]], writes=[B_PT[sl]])

    def na_B(step):
        r, g = na_steps[step]
        rs = min(max(r - 4, 0), 24)
        sl = step % 2
        ys = r % 2
        pvb = 4 + sl
        for hh in range(4):
            h = g * 4 + hh
            for kt in range(4):
                if rs % 2 == 0:
                    vsrc, bv, ti = Va, B_VA, rs // 2 + kt
                else:
                    vsrc, bv, ti = Vas, B_VAS, (rs - 1) // 2 + kt
                P.op("pe", lambda e, pvb=pvb, hh=hh, h=h, kt=kt, sl=sl, vsrc=vsrc, ti=ti: e.matmul(
                    ps[0:64, pvb * 512 + hh * 65:pvb * 512 + (hh + 1) * 65],
                    lhsT=PT[sl][:, kt * 256 + hh * 64:kt * 256 + (hh + 1) * 64], rhs=vsrc[:, ti, h, :],
                    start=(kt == 0), stop=(kt == 3)),
                    reads=[B_PT[sl], bv], writes=[PB[pvb]], sig=(hh == 3 and kt == 3))
        P.op("dve", lambda e, pvb=pvb, sl=sl: e.reciprocal(out=rec[0:64, sl, :], in_=cap(psA, pvb * 512 + 64, [[65, 4]], parts=64)),
             reads=[PB[pvb]], writes=[B_REC[sl]])
        P.op("dve", lambda e, pvb=pvb, sl=sl, g=g, ys=ys: e.tensor_tensor(
            out=yar[ys][0:64, g * 256:(g + 1) * 256].rearrange("p (h d) -> p h d", h=4),
            in0=cap(psA, pvb * 512, [[65, 4], [1, 64]], parts=64),
            in1=cap(rec, sl * 4, [[1, 4], [0, 64]], parts=64), op=ALU.mult),
            reads=[PB[pvb], B_REC[sl]], wdis=[B_YAR[ys]])
        if g == 1:
            tb = 6 + ys
            pbt = bankb(tb)
            for c in range(4):
                P.op("pe", lambda e, c=c, ys=ys, pbt=pbt: e.transpose(pbt[:, c * 64:(c + 1) * 64], yar[ys][0:64, c * 128:(c + 1) * 128], identb[0:64, 0:64]),
                     reads=[B_YAR[ys], B_C], writes=[PB[tb]], sig=(c == 3))
            P.op("act", lambda e, r=r, pbt=pbt: e.activation(out=yaT[:, :, r * 64:(r + 1) * 64], in_=pbt[:, 0:256].rearrange("p (c t) -> p c t", c=4), func=AF.Copy),
                 reads=[PB[tb]], writes=[B_YAT])

    na_A(0)
    for step in range(64):
        if step + 1 < 64:
            na_A(step + 1)
        na_B(step)

    def dump_fm(name, srcT, B_src, nch):
        if name in dbg_d:
            tmpf = A.alloc([1024], F32)
            B_T = Buf("tmpf_" + name)
            for k in range(nch):
                for hf in range(2):
                    P.op("dve", lambda e, k=k, hf=hf: e.tensor_copy(out=tmpf, in_=srcT[:, k, hf * 1024:(hf + 1) * 1024]), reads=[B_src], writes=[B_T])
                    dbg_out(name, tmpf, [B_T], k * 128, 128, hf * 1024, 1024)

    dump_fm("yaT", yaT, B_YAT, 4)
    if stage <= 1:
        P.emit(finals)
        return nc, es

    P.barrier()
    A.top = ph_mark
    ybT = A.alloc([4, S], BF16)
    if dbg_d:
        P.op("pool", lambda e: e.memset(ybT.rearrange("p a b -> p (a b)"), 0.0), writes=[B_YBT])
    mg_mark = A.top
    QbT = A.alloc([4, S], BF16)
    KbT = A.alloc([4, S], BF16)
    Vb = A.alloc([16, 2, 65], BF16)
    ropeC = A.alloc([S], F32)
    ropeS = A.alloc([S], F32)
    permT = A.alloc([128], F32)
    blk64 = A.alloc([128], F32)
    gqv = A.alloc([1], F32)
    gkv = A.alloc([1], F32)
    B_QB, B_KB, B_VB, B_RC = Buf("QbT"), Buf("KbT"), Buf("Vb"), Buf("ropec")
    for dst, src, key in ((ropeC, ropec_d, 20), (ropeS, ropes_d, 21), (permT, perm_d, 22), (blk64, blk_d, 23), (gqv, gq_d, 24), (gkv, gk_d, 25)):
        P.op("sp", lambda e, dst=dst, src=src: e.dma_start(out=dst, in_=src.ap()), writes=[B_RC], dma=True, semkey=("c", key))
    P.op("pool", lambda e: e.memset(Vb.rearrange("p a b c -> p (a b c)"), 1.0), writes=[B_VB])
    P.op("pool", lambda e: e.memset(KbT.rearrange("p a b -> p (a b)"), 0.0), writes=[B_KB])
    sqf = A.alloc([512], F32)
    rtf = A.alloc([512], F32)
    rsf = A.alloc([512], F32)
    qn = A.alloc([512], F32)
    t1 = A.alloc([512], F32)
    t2 = A.alloc([512], F32)
    B_SQ, B_RT, B_RS, B_QN, B_T1, B_T2 = (Buf(n) for n in ("sqf", "rtf", "rsf", "qn", "t1", "t2"))

    def qk_post(dstT, B_dst, gvec, kmode=False):
        def post(c, tc, b):
            b2 = next_bank()
            P.op("act", lambda e: e.activation(out=sqf, in_=bank(b), func=AF.Square), reads=[PB[b]], writes=[B_SQ])
            P.op("pe", lambda e: e.matmul(bank(b2), lhsT=blk64, rhs=sqf, start=True, stop=True), reads=[B_SQ, B_RC], writes=[PB[b2]])
            P.op("act", lambda e: e.activation(out=rtf, in_=bank(b2), func=AF.Sqrt, bias=EPS), reads=[PB[b2]], writes=[B_RT])
            P.op("dve", lambda e: e.reciprocal(out=rsf, in_=rtf), reads=[B_RT], writes=[B_RS])
            P.op("dve", lambda e: e.scalar_tensor_tensor(out=qn, in0=bank(b), scalar=gvec[:, 0:1], in1=rsf, op0=ALU.mult, op1=ALU.mult),
                 reads=[PB[b], B_RS, B_RC], writes=[B_QN])
            b3 = next_bank()
            P.op("pe", lambda e: e.matmul(bank(b3), lhsT=permT, rhs=qn, start=True, stop=True), reads=[B_QN, B_RC], writes=[PB[b3]])
            P.op("dve", lambda e: e.tensor_tensor(out=t1, in0=qn, in1=ropeC[:, tc * 512:(tc + 1) * 512], op=ALU.mult), reads=[B_QN, B_RC], writes=[B_T1])
            P.op("dve", lambda e: e.tensor_tensor(out=t2, in0=bank(b3), in1=ropeS[:, tc * 512:(tc + 1) * 512], op=ALU.mult), reads=[PB[b3], B_RC], writes=[B_T2])
            if not kmode:
                P.op("pool", lambda e: e.tensor_tensor(out=dstT[:, c, tc * 512:(tc + 1) * 512], in0=t1, in1=t2, op=ALU.add), reads=[B_T1, B_T2], writes=[B_dst])
            else:
                lo_idx = 0 if c == 0 else 2
                hi_idx = 3 if c == 0 else 1
                P.op("pool", lambda e: e.tensor_tensor(out=dstT[0:64, lo_idx, tc * 512:(tc + 1) * 512], in0=t1[0:64, :], in1=t2[0:64, :], op=ALU.add), reads=[B_T1, B_T2], writes=[B_dst])
                P.op("pool", lambda e: e.tensor_tensor(out=dstT[64:128, hi_idx, tc * 512:(tc + 1) * 512], in0=t1[64:128, :], in1=t2[64:128, :], op=ALU.add), reads=[B_T1, B_T2], writes=[B_dst])
        return post

    load_w(0, win_d, 4352, 2048, 128, dcol=0)
    load_w(0, win_d, 4352, 2112, 64, dcol=128)
    load_w(0, win_d, 4352, 2048, 64, dcol=192)
    load_w(0, win_d, 4352, 2176, 128, dcol=256)
    proj_fm(1, 512, QbT, B_QB, post=qk_post(QbT, B_QB, gqv))
    proj_fm(0, 256, KbT, B_KB, post=qk_post(KbT, B_KB, gkv, kmode=True))
    for i in range(16):
        b = next_bank()
        for k in range(8):
            P.op("pe", lambda e, k=k, b=b, i=i: e.matmul(bank(b, 0, 128), lhsT=hT[:, k, i * 128:(i + 1) * 128], rhs=wbf[0][:, k, 256:384],
                                                      start=(k == 0), stop=(k == 7)), reads=[B_WB[0]] + B_HT, writes=[PB[b]], sig=(k == 7))
        evac(Vb[:, i, :, 0:64], bank(b, 0, 128).rearrange("p (h d) -> p h d", d=64), [PB[b]], [B_VB])

    dump_fm("QbT", QbT, B_QB, 4)
    dump_fm("KbT", KbT, B_KB, 4)

    PTf = [A.alloc([16, 512], BF16) for _ in range(2)]
    B_PTF = [Buf("ptf0"), Buf("ptf1")]
    ybt = A.alloc([4, 512], BF16)
    B_YBTOK = Buf("ybtok")
    recg = A.alloc([2, 4], F32)
    B_RECG = [Buf("recg0"), Buf("recg1")]
    _sg = [0]

    def emit_S(hc, kb2):
        qc, h = hc // 8, hc % 8
        sl = _sg[0] % 2
        _sg[0] += 1
        hs = hc % 2
        kvh = h // 4
        for j in range(2):
            kt = kb2 * 2 + j
            bk = sl * 2 + j
            P.op("pe", lambda e, bk=bk, kvh=kvh, kt=kt, h=h, qc=qc: e.matmul(
                bank(bk), lhsT=KbT[:, kvh * 2 + (h % 2), kt * 128:(kt + 1) * 128],
                rhs=QbT[:, h // 2, qc * 512:(qc + 1) * 512], start=True, stop=True),
                reads=[B_KB, B_QB], writes=[PB[bk]])
        P.op("act", lambda e, sl=sl, hs=hs, kb2=kb2: e.activation(out=PTf[hs][:, kb2 * 2:kb2 * 2 + 2, :].rearrange("p a b -> p (a b)"),
                                                            in_=ps[:, sl * 1024:(sl + 1) * 1024], func=AF.Exp, scale=0.125),
             reads=[PB[sl * 2], PB[sl * 2 + 1]], wdis=[B_PTF[hs]])

    def emit_PVall(hc):
        qc, h = hc // 8, hc % 8
        hs = hc % 2
        kvh = h // 4
        pvb = 4 + hs
        for qt in range(4):
            for kt in range(16):
                P.op("pe", lambda e, pvb=pvb, qt=qt, hs=hs, kt=kt, kvh=kvh: e.matmul(
                    ps[:, pvb * 512 + qt * 65:pvb * 512 + (qt + 1) * 65], lhsT=PTf[hs][:, kt, qt * 128:(qt + 1) * 128],
                    rhs=Vb[:, kt, kvh, :], start=(kt == 0), stop=(kt == 15)),
                    reads=[B_PTF[hs], B_VB], writes=[PB[pvb]], sig=(qt == 3 and kt == 15))
        P.op("dve", lambda e, pvb=pvb, hs=hs: e.reciprocal(out=recg[:, hs, :], in_=cap(psA, pvb * 512 + 64, [[65, 4]])),
             reads=[PB[pvb]], writes=[B_RECG[hs]])
        P.op("dve", lambda e, pvb=pvb, hs=hs, h=h: e.tensor_tensor(
            out=ybt[:, :, h * 64:(h + 1) * 64], in0=cap(psA, pvb * 512, [[65, 4], [1, 64]]),
            in1=cap(recg, hs * 4, [[1, 4], [0, 64]]), op=ALU.mult),
            reads=[PB[pvb], B_RECG[hs]], writes=[B_YBTOK])
        if h == 7:
            for qt in range(4):
                tb = 6 + qt % 2
                pbt = bankb(tb)
                for c in range(4):
                    P.op("pe", lambda e, c=c, qt=qt, pbt=pbt: e.transpose(pbt[:, c * 128:(c + 1) * 128], ybt[:, qt, c * 128:(c + 1) * 128], identb),
                         reads=[B_YBTOK, B_C], writes=[PB[tb]], sig=(c == 3))
                t0 = qc * 512 + qt * 128
                P.op("act", lambda e, t0=t0, pbt=pbt: e.activation(out=ybT[:, :, t0:t0 + 128], in_=pbt[:, 0:512].rearrange("p (c t) -> p c t", c=4), func=AF.Copy),
                     reads=[PB[tb]], writes=[B_YBT])

    for hc in range(33):
        if hc < 32:
            for kb2 in range(8):
                emit_S(hc, kb2)
                if kb2 == 1 and hc > 0:
                    emit_PVall(hc - 1)
        else:
            emit_PVall(31)

    dump_fm("ybT", ybT, B_YBT, 4)
    if stage <= 2:
        P.emit(finals)
        return nc, es


    P.barrier()
    A.top = mg_mark
    mT = A.alloc([8, S], BF16)
    B_MT = Buf("mT")
    md_mark = A.top
    wpa = A.alloc([4, D], BF16)
    wpb = A.alloc([4, D], BF16)
    B_WP = Buf("wp")
    P.op("pool", lambda e: e.dma_start(out=wpa, in_=dap(wpa_d, 0, [[D, 128], [128 * D, 4], [1, D]])), writes=[B_WP], dma=True, semkey=("c", 30))
    P.op("pool", lambda e: e.dma_start(out=wpb, in_=dap(wpb_d, 0, [[D, 128], [128 * D, 4], [1, D]])), writes=[B_WP], dma=True, semkey=("c", 31))
    sga = A.alloc([512], BF16)
    sgb = A.alloc([512], BF16)
    m1 = A.alloc([512], F32)
    m2 = A.alloc([512], F32)
    B_SGA, B_SGB, B_M1, B_M2 = Buf("sga"), Buf("sgb"), Buf("m1"), Buf("m2")
    for grp in range(2):
        load_w(0, win_d, 4352, 2304 + grp * 512, 512)
        load_w(1, win_d, 4352, 3328 + grp * 512, 512)
        for cn in range(4):
            n = grp * 4 + cn
            for tc in range(4):
                for (slot, wp, yT, B_y, sg, B_sg, mm, B_mm) in ((0, wpa, yaT, B_YAT, sga, B_SGA, m1, B_M1), (1, wpb, ybT, B_YBT, sgb, B_SGB, m2, B_M2)):
                    b = next_bank()
                    for k in range(8):
                        P.op("pe", lambda e, k=k, b=b, slot=slot, cn=cn, tc=tc: e.matmul(bank(b), lhsT=wbf[slot][:, k, cn * 128:(cn + 1) * 128],
                                                                                  rhs=hT[:, k, tc * 512:(tc + 1) * 512], start=(k == 0), stop=(k == 7)),
                             reads=[B_WB[slot], B_HT[tc]], writes=[PB[b]], sig=(k == 7))
                    P.op("act", lambda e, b=b, sg=sg: e.activation(out=sg, in_=bank(b), func=AF.Sigmoid), reads=[PB[b]], writes=[B_sg])
                    b2 = next_bank()
                    for c in range(4):
                        P.op("pe", lambda e, c=c, b2=b2, wp=wp, yT=yT, n=n, tc=tc: e.matmul(bank(b2), lhsT=wp[:, c, n * 128:(n + 1) * 128],
                                                                                      rhs=yT[:, c, tc * 512:(tc + 1) * 512], start=(c == 0), stop=(c == 3)),
                             reads=[B_WP, B_y], writes=[PB[b2]], sig=(c == 3))
                    P.op("dve", lambda e, b2=b2, sg=sg, mm=mm: e.tensor_tensor(out=mm, in0=sg, in1=bank(b2), op=ALU.mult), reads=[PB[b2], B_sg], writes=[B_mm])
                P.op("pool", lambda e, n=n, tc=tc: e.tensor_tensor(out=mT[:, n, tc * 512:(tc + 1) * 512], in0=m1, in1=m2, op=ALU.add),
                     reads=[B_M1, B_M2], writes=[B_MT])
    dump_fm("mT", mT, B_MT, 8)
    if stage <= 2.5:
        P.emit(finals)
        return nc, es

    P.barrier()
    A.top = md_mark
    xnT = hT
    B_XNT = B_HT
    wo = A.alloc([8, D], BF16)
    B_WO = Buf("wo")
    for kh in range(4):
        P.op("pool", lambda e, kh=kh: e.dma_start(out=wo[:, kh * 2:(kh + 1) * 2, :], in_=dap(wo_d, kh * 2 * 128 * D, [[D, 128], [128 * D, 2], [1, D]])),
             writes=[B_WO], dma=True, semkey=("c", 32))
    g2b = A.alloc([D], F32)
    P.op("sp", lambda e: e.dma_start(out=g2b, in_=dap(g2_d, 0, [[0, 128], [1, D]])), writes=[B_C], dma=True, semkey=("c", 33))
    xt2 = [A.alloc([D], F32) for _ in range(2)]
    x1t = [A.alloc([D], F32) for _ in range(2)]
    xnb = [A.alloc([D], BF16) for _ in range(2)]
    junk_ref[0] = A.alloc([D], BF16)
    B_XT2 = [Buf("xt2_0"), Buf("xt2_1")]
    B_X1T = [Buf("x1t0"), Buf("x1t1")]
    B_XNB = [Buf("xnb0"), Buf("xnb1")]
    B_X1S = [Buf("x1s%d" % i) for i in range(NT)]
    for i in range(NT):
        s_ = i % 2
        P.op("sp", lambda e, i=i, s_=s_: e.dma_start(out=xt2[s_], in_=x_d.ap()[i * 128:(i + 1) * 128, :]), writes=[B_XT2[s_]], dma=True, semkey=("xt2", s_))
        for half in range(2):
            b = next_bank()
            for k in range(8):
                P.op("pe", lambda e, k=k, b=b, i=i, half=half: e.matmul(bank(b), lhsT=mT[:, k, i * 128:(i + 1) * 128], rhs=wo[:, k, half * 512:(half + 1) * 512],
                                                                    start=(k == 0), stop=(k == 7)), reads=[B_MT, B_WO], writes=[PB[b]], sig=(k == 7))
            P.op("dve", lambda e, b=b, s_=s_, half=half: e.tensor_tensor(out=x1t[s_][:, half * 512:(half + 1) * 512], in0=xt2[s_][:, half * 512:(half + 1) * 512],
                                                                   in1=bank(b), op=ALU.add), reads=[PB[b], B_XT2[s_]], writes=[B_X1T[s_]])
        P.op("sp", lambda e, i=i, s_=s_: e.dma_start(out=x1s_d.ap()[i * 128:(i + 1) * 128, :], in_=x1t[s_]), reads=[B_X1T[s_]], writes=[B_X1S[i]],
             dma=True, semkey=("x1t", s_))
        rms_tile(i, x1t[s_], g2b, s_, [B_X1T[s_]], B_XNB[s_], xnb[s_])
        transpose_tile_to(i, xnb[s_], B_XNB[s_], xnT, B_XNT[i // 4], 6 + s_)
        if "x1" in dbg_d:
            dbg_out("x1", x1t[s_], [B_X1T[s_]], i * 128, 128, 0, D)
    if "xnT" in dbg_d:
        tmpf = A.alloc([1024], F32)
        B_T = Buf("tmpf_xn")
        for k in range(8):
            for hf in range(2):
                P.op("dve", lambda e, k=k, hf=hf: e.tensor_copy(out=tmpf, in_=xnT[:, k, hf * 1024:(hf + 1) * 1024]), reads=B_XNT, writes=[B_T])
                dbg_out("xnT", tmpf, [B_T], k * 128, 128, hf * 1024, 1024)
    if stage <= 3:
        P.emit(finals)
        return nc, es

    P.barrier()
    A.top = wbf_mark
    abgT = A.alloc([3, S], BF16)
    B_ABG = Buf("abgT")
    pk_mark = A.top
    A.top = att_mark
    wq = A.alloc([8, 2048], BF16)
    B_WQ = Buf("wq")
    for q4 in range(4):
        P.op("pool", lambda e, q4=q4: e.dma_start(out=wq[:, :, q4 * 512:(q4 + 1) * 512], in_=dap(wq_d, q4 * 512, [[2048, 128], [128 * 2048, 8], [1, 512]])),
             writes=[B_WQ], dma=True, semkey=("c", 40))
    skf = A.alloc([16, 128], F32)
    keysT = A.alloc([16, 128], BF16)
    B_SK, B_KT = Buf("skf"), Buf("keysT")
    P.op("sp", lambda e: e.dma_start(out=skf, in_=dap(sk_d, 0, [[128, 128], [128 * 128, 16], [1, 128]])), writes=[B_SK], dma=True, semkey=("c", 41))
    for hp in range(16):
        b = 4 + hp % 2
        P.op("pe", lambda e, hp=hp, b=b: e.transpose(bank(b, 0, 128), skf[:, hp, :], identf), reads=[B_SK, B_C], writes=[PB[b]])
        evac(keysT[:, hp, :], bank(b, 0, 128), [PB[b]], [B_KT])
    if stage <= 3.2:
        P.emit(finals)
        return nc, es
    qT = A.alloc([16, 512], BF16)
    B_QT = Buf("qT")
    sc = A.alloc([16, 128], F32)
    B_SC = Buf("sc")
    tv = A.alloc([16, 16], F32)
    tix = A.alloc([16, 16], U32)
    tixf = A.alloc([16, 16], F32)
    tmp1 = A.alloc([128], F32)
    cand = A.alloc([8, 256], F32)
    tmp2 = A.alloc([256], F32)
    bs = A.alloc([8, 16], F32)
    pos = A.alloc([8, 16], U32)
    k1u = A.alloc([128], U32)
    k2u = A.alloc([128], U32)
    k1f = A.alloc([128], F32)
    k2f = A.alloc([128], F32)
    eq1 = A.alloc([128, 16], F32)
    eq2 = A.alloc([128, 16], F32)
    abg = A.alloc([3, 128], F32)
    ee = A.alloc([8, 16], F32)
    zz = A.alloc([2, 8], F32)
    B_TK, B_PL, B_AB, B_EE = Buf("topk"), Buf("poolgather"), Buf("abg"), Buf("ee")

    ub = [A.alloc([D], BF16) for _ in range(2)]
    usb = [A.alloc([8, 128], BF16) for _ in range(2)]
    B_UB = [Buf("ub0"), Buf("ub1")]
    B_USB = [Buf("usb0"), Buf("usb1")]
    B_VS = [Buf("vs%d" % i) for i in range(32)]
    B_VSK = [Buf("vsk%d" % i) for i in range(4)]
    B_UTS = [Buf("uts%d" % i) for i in range(128)]

    def prepass_chunk(ec):
        if ec % 4 == 0:
            c = ec // 4
            P.op("pool", lambda e, c=c: e.dma_start(out=vs_d.ap()[c * 512:(c + 1) * 512, :], in_=v_d.ap()[c * 512:(c + 1) * 512, :]),
                 writes=[B_VS[c], B_VSK[c % 4]], dma=True, semkey=("vs", c % 4))
        s_ = ec % 2
        P.op("pool", lambda e, ec=ec, s_=s_: e.dma_start(out=ub[s_], in_=u_d.ap()[ec * 128:(ec + 1) * 128, :]), writes=[B_UB[s_]], dma=True, semkey=("ub", s_))
        b = 6 + s_
        pb = bankb(b)
        for k in range(8):
            P.op("pe", lambda e, k=k, s_=s_, pb=pb: e.transpose(pb[:, k * 128:(k + 1) * 128], ub[s_][:, k * 128:(k + 1) * 128], identb),
                 reads=[B_UB[s_], B_C], writes=[PB[b]], sig=(k == 7))
        evac(usb[s_].rearrange("p a b -> p (a b)"), pb, [PB[b]], [B_USB[s_]], eng="act")
        P.op("sp", lambda e, ec=ec, s_=s_: e.dma_start(out=dap(uts_d, ec * 131072, [[1024, 128], [1, 1024]]), in_=usb[s_].rearrange("p a b -> p (a b)")),
             reads=[B_USB[s_]], writes=[B_UTS[ec]], dma=True, semkey=("usb", s_))

    for tc in range(4):
        for hp in range(16):
            b = 4 + hp % 2
            for k in range(8):
                P.op("pe", lambda e, k=k, b=b, hp=hp, tc=tc: e.matmul(bank(b), lhsT=wq[:, k, hp * 128:(hp + 1) * 128], rhs=xnT[:, k, tc * 512:(tc + 1) * 512],
                                                                 start=(k == 0), stop=(k == 7)), reads=[B_WQ, B_XNT[tc]], writes=[PB[b]], sig=(k == 7))
            evac(qT[:, hp, :], bank(b), [PB[b]], [B_QT])
        for j in range(4):
            i = tc * 4 + j
            for hp in range(16):
                P.op("pe", lambda e, hp=hp, j=j: e.matmul(ps[:, hp * 128:(hp + 1) * 128], lhsT=qT[:, hp, j * 128:(j + 1) * 128], rhs=keysT[:, hp, :], start=True, stop=True),
                     reads=[B_QT, B_KT], writes=[PB[hp // 4]], sig=(hp == 15))
            P.op("act", lambda e: e.activation(out=sc.rearrange("p a b -> p (a b)"), in_=ps[:, 0:2048], func=AF.Copy), reads=PB[0:4], writes=[B_SC])
            if "sc" in dbg_d:
                dbg_out("sc", sc.rearrange("p a b -> p (a b)"), [B_SC], i * 128, 128, 0, 2048)
            if stage <= 3.4:
                P.emit(finals)
                return nc, es
            for cc in range(8):
                prepass_chunk(i * 8 + cc)
            for hp in range(16):
                P.op("dve", lambda e, hp=hp: e.max(out=tv[:, hp, 0:8], in_=sc[:, hp, :]), reads=[B_SC], writes=[B_TK])
                P.op("dve", lambda e, hp=hp: e.max_index(out=tix[:, hp, 0:8], in_max=tv[:, hp, 0:8], in_values=sc[:, hp, :]), reads=[B_SC, B_TK], writes=[B_TK])
                P.op("dve", lambda e, hp=hp: e.match_replace(out=tmp1, in_to_replace=tv[:, hp, 0:8], in_values=sc[:, hp, :], imm_value=-1e30), reads=[B_SC, B_TK], writes=[B_TK])
                P.op("dve", lambda e, hp=hp: e.max(out=tv[:, hp, 8:16], in_=tmp1), reads=[B_TK], writes=[B_TK])
                P.op("dve", lambda e, hp=hp: e.max_index(out=tix[:, hp, 8:16], in_max=tv[:, hp, 8:16], in_values=tmp1), reads=[B_TK], writes=[B_TK])
            if stage <= 3.5:
                P.emit(finals)
                return nc, es
            P.op("pool", lambda e: e.tensor_copy(out=tixf.rearrange("p a b -> p (a b)"), in_=tix.rearrange("p a b -> p (a b)")), reads=[B_TK], writes=[B_PL])
            if stage <= 3.6:
                P.emit(finals)
                return nc, es
            for h in range(8):
                P.op("dve", lambda e, h=h: e.tensor_tensor(out=cand[:, h, :].rearrange("p (a b) -> p a b", a=16),
                                                           in0=cap(tv, (2 * h) * 16, [[1, 16], [0, 16]]), in1=cap(tv, (2 * h + 1) * 16, [[0, 16], [1, 16]]), op=ALU.add),
                     reads=[B_TK], writes=[B_TK])
                P.op("dve", lambda e, h=h: e.max(out=bs[:, h, 0:8], in_=cand[:, h, :]), reads=[B_TK], writes=[B_TK])
                P.op("dve", lambda e, h=h: e.max_index(out=pos[:, h, 0:8], in_max=bs[:, h, 0:8], in_values=cand[:, h, :]), reads=[B_TK], writes=[B_TK])
                P.op("dve", lambda e, h=h: e.match_replace(out=tmp2, in_to_replace=bs[:, h, 0:8], in_values=cand[:, h, :], imm_value=-1e30), reads=[B_TK], writes=[B_TK])
                P.op("dve", lambda e, h=h: e.max(out=bs[:, h, 8:16], in_=tmp2), reads=[B_TK], writes=[B_TK])
                P.op("dve", lambda e, h=h: e.max_index(out=pos[:, h, 8:16], in_max=bs[:, h, 8:16], in_values=tmp2), reads=[B_TK], writes=[B_TK])
            posf = pos.rearrange("p a b -> p (a b)")
            P.op("dve", lambda e: e.tensor_single_scalar(out=k1u, in_=posf, scalar=4, op=ALU.logical_shift_right), reads=[B_TK], writes=[B_TK])
            P.op("dve", lambda e: e.tensor_single_scalar(out=k2u, in_=posf, scalar=15, op=ALU.bitwise_and), reads=[B_TK], writes=[B_TK])
            P.op("dve", lambda e: e.tensor_copy(out=k1f, in_=k1u), reads=[B_TK], writes=[B_TK])
            P.op("dve", lambda e: e.tensor_copy(out=k2f, in_=k2u), reads=[B_TK], writes=[B_TK])
            if stage <= 3.7:
                P.emit(finals)
                return nc, es
            for (kf, eq, pp, dst) in ((k1f, eq1, 0, 0), (k2f, eq2, 1, 1)):
                P.op("dve", lambda e, kf=kf, eq=eq: e.tensor_tensor(out=eq, in0=cap(iota16, 0, [[0, 128], [1, 16]]), in1=cap(kf, 0, [[1, 128], [0, 16]]), op=ALU.is_equal),
                     reads=[B_TK, B_C, B_AB], writes=[B_PL])
                for h in range(8):
                    P.op("pool", lambda e, eq=eq, h=h, pp=pp: e.tensor_tensor(out=eq[:, h * 16:(h + 1) * 16, :], in0=eq[:, h * 16:(h + 1) * 16, :],
                                                                        in1=cap(tixf, (2 * h + pp) * 16, [[0, 16], [1, 16]]), op=ALU.mult), reads=[B_PL], writes=[B_PL])
                P.op("dve", lambda e, eq=eq, dst=dst: e.tensor_reduce(out=abg[:, dst, :], in_=eq, axis=mybir.AxisListType.X, op=ALU.add), reads=[B_PL], writes=[B_AB])
            if stage <= 3.8:
                P.emit(finals)
                return nc, es
            P.op("dve", lambda e: e.tensor_tensor(out=ee, in0=bs, in1=cap(bs, 0, [[16, 8], [0, 16]]), op=ALU.subtract), reads=[B_TK], writes=[B_EE])
            P.op("act", lambda e: e.activation(out=ee, in_=ee, func=AF.Exp), reads=[B_EE], writes=[B_EE])
            P.op("dve", lambda e: e.tensor_reduce(out=zz[:, 0, :], in_=ee, axis=mybir.AxisListType.X, op=ALU.add), reads=[B_EE], writes=[B_EE])
            P.op("dve", lambda e: e.reciprocal(out=zz[:, 1, :], in_=zz[:, 0, :]), reads=[B_EE], writes=[B_EE])
            P.op("dve", lambda e: e.tensor_tensor(out=abg[:, 2, :].rearrange("p (h j) -> p h j", h=8), in0=ee, in1=cap(zz, 8, [[1, 8], [0, 16]]), op=ALU.mult),
                 reads=[B_EE], writes=[B_AB])
            if "abg" in dbg_d:
                dbg_out("abg", abg.rearrange("p a b -> p (a b)"), [B_AB], i * 128, 128, 0, 384)
            if stage <= 3.9:
                P.emit(finals)
                return nc, es
            tb = 6 + i % 2
            for q in range(3):
                P.op("pe", lambda e, q=q, tb=tb: e.transpose(bank(tb, q * 128, (q + 1) * 128), abg[:, q, :], identf), reads=[B_AB, B_C], writes=[PB[tb]], sig=(q == 2))
            P.op("act", lambda e, tb=tb, i=i: e.activation(out=abgT[:, :, i * 128:(i + 1) * 128], in_=bank(tb, 0, 384).rearrange("p (q t) -> p q t", q=3), func=AF.Copy),
                 reads=[PB[tb]], writes=[B_ABG])
            if stage <= 3.95 and i + 1 >= PK_TILES:
                P.emit(finals)
                return nc, es
    if stage <= 4:
        P.emit(finals)
        return nc, es

    P.barrier()
    A.top = pk_mark
    GTh = [A.alloc([64, 256], BF16) for _ in range(2)]
    B_GTH = [Buf("GTlo"), Buf("GThi")]
    RAh = [A.alloc([16, 64], BF16) for _ in range(2)]
    LBe = [A.alloc([16, 128], BF16) for _ in range(2)]
    LB = [A.alloc([16, 128], BF16) for _ in range(2)]
    B_RA = [Buf("RA0"), Buf("RA1")]
    B_LBE = [Buf("LBe0"), Buf("LBe1")]
    B_LB = [Buf("LB0"), Buf("LB1")]
    NSB = 3
    uTb = [A.alloc([4, 8, 128], BF16) for _ in range(NSB)]
    vbuf = [A.alloc([4, D], BF16) for _ in range(NSB)]
    B_UTB = [Buf("utb%d" % i) for i in range(NSB)]
    B_VBUF = [Buf("vbuf%d" % i) for i in range(NSB)]
    B_GS = [Buf("gslot%d" % i) for i in range(4)]
    gl = [A.alloc([2, 256], BF16) for _ in range(2)]
    B_GL = [Buf("gl0"), Buf("gl1")]
    hidT = [A.alloc([256], BF16) for _ in range(4)]
    B_HID = [Buf("hid%d" % i) for i in range(4)]
    x2t = [A.alloc([D], F32) for _ in range(2)]
    B_X2 = [Buf("x2t0"), Buf("x2t1")]
    yt = A.alloc([D], F32)
    B_YT = Buf("yt")
    gfb = A.alloc([D], F32)
    junk_ref[0] = A.alloc([D], BF16)
    P.op("sp", lambda e: e.dma_start(out=gfb, in_=dap(gf_d, 0, [[0, 128], [1, D]])), writes=[B_C], dma=True, semkey=("c", 50))
    B_OUT = [Buf("out%d" % i) for i in range(NT)]
    NB = 8
    _gq = [0]
    _gbk = [0]

    def build_ra(X, half, s_):
        q_ = s_ % 2
        t0 = X * 256 + s_ * 16
        P.op("dve", lambda e, t0=t0, q_=q_, half=half: e.tensor_tensor(out=RAh[q_], in0=cap(iotab, half * 64, [[0, 16], [1, 64]]),
                                                                 in1=cap(abgT, 0 * S + t0, [[1, 16], [0, 64]]), op=ALU.is_equal),
             reads=[B_ABG, B_C], writes=[B_RA[q_]])

    def build_lb(X, half, s_):
        q_ = s_ % 2
        t0 = X * 256 + s_ * 16
        P.op("dve", lambda e, t0=t0, q_=q_: e.tensor_tensor(out=LBe[q_], in0=cap(iotab, 0, [[0, 16], [1, 128]]), in1=cap(abgT, 1 * S + t0, [[1, 16], [0, 128]]), op=ALU.is_equal),
             reads=[B_ABG, B_C], writes=[B_LBE[q_]])
        P.op("pool", lambda e, t0=t0, q_=q_: e.tensor_tensor(out=LB[q_], in0=LBe[q_], in1=cap(abgT, 2 * S + t0, [[1, 16], [0, 128]]), op=ALU.mult),
             reads=[B_ABG, B_LBE[q_]], writes=[B_LB[q_]])

    def build_vec(X, half, s_):
        build_ra(X, half, s_)
        build_lb(X, half, s_)

    def build_pe(X, half, s_):
        q_ = s_ % 2
        for t8 in range(2):
            gb = t8
            for tq in range(8):
                t = t8 * 8 + tq
                P.op("pe", lambda e, t=t, gb=gb, tq=tq, q_=q_: e.matmul(bank(gb, tq * 64, (tq + 1) * 64), lhsT=LB[q_][:, t, :], rhs=RAh[q_][:, t, :], start=True, stop=True),
                     reads=[B_LB[q_], B_RA[q_]], writes=[PB[gb]], sig=(tq == 7))
            tl = s_ * 16 + t8 * 8
            evac(cap(GTh[half], tl, [[256, 64], [1, 8]]), cap(psA, gb * 512, [[1, 64], [64, 8]]), [PB[gb]], [B_GTH[half]], eng="act")

    for s_ in range(16):
        build_vec(0, 0, s_)
        build_pe(0, 0, s_)

    pend = [None]
    for blk in range(NB):
        tb0 = blk * 256

        def emit_act(pr):
            ab = 2 + pr % 2
            for hf in range(2):
                i1 = pr * 2 + hf
                grp, ec = i1 // 4, i1 % 4
                s_ = (blk * 32 + grp) % NSB
                if ec == 0:
                    P.op("sp", lambda e, grp=grp, s_=s_: e.dma_start(out=uTb[s_].rearrange("p a k e -> p a (k e)"),
                                                                  in_=dap(uts_d, grp * 4 * 131072, [[1024, 128], [131072, 4], [1, 1024]])),
                         reads=B_UTS[grp * 4:(grp + 1) * 4], writes=[B_UTB[s_]], dma=True, semkey=("utb", s_))
                    P.op("sp", lambda e, grp=grp, s_=s_: e.dma_start(out=vbuf[s_], in_=dap(vs_d, grp * 512 * 1024, [[1024, 128], [128 * 1024, 4], [1, 1024]])),
                         reads=[B_VS[grp]], writes=[B_VBUF[s_]], dma=True, semkey=("vbuf", s_))
                for k in range(8):
                    P.op("pe", lambda e, k=k, ab=ab, hf=hf, ec=ec, s_=s_, tb0=tb0: e.matmul(bank(ab, hf * 256, (hf + 1) * 256), lhsT=uTb[s_][:, ec, k, :],
                                                                                      rhs=xnT[:, k, tb0:tb0 + 256], start=(k == 0), stop=(k == 7)),
                         reads=[B_UTB[s_], B_XNT[tb0 // 512]], writes=[PB[ab]], sig=(k == 7))

        def emit_out(pr):
            ab = 2 + pr % 2
            gs = pr % 2
            P.op("act", lambda e, ab=ab, gs=gs: e.activation(out=gl[gs].rearrange("p a b -> p (a b)"), in_=bank(ab), func=AF.Gelu), reads=[PB[ab]], writes=[B_GL[gs]])
            for hf in range(2):
                i1 = pr * 2 + hf
                grp, ec = i1 // 4, i1 % 4
                s_ = (blk * 32 + grp) % NSB
                hs = i1 % 4
                gh = i1 // 64
                P.op("dve", lambda e, gs=gs, hf=hf, i1=i1, hs=hs, gh=gh: e.tensor_tensor(out=hidT[hs], in0=gl[gs][:, hf, :], in1=GTh[gh][:, i1 % 64, :], op=ALU.mult),
                     reads=[B_GL[gs], B_GTH[gh]], writes=[B_HID[hs]])
                for tt in range(2):
                    for h2 in range(2):
                        ob = 4 + tt * 2 + h2
                        P.op("pe", lambda e, ob=ob, hs=hs, tt=tt, h2=h2, ec=ec, s_=s_, i1=i1: e.matmul(
                            bank(ob), lhsT=hidT[hs][:, tt * 128:(tt + 1) * 128], rhs=vbuf[s_][:, ec, h2 * 512:(h2 + 1) * 512],
                            start=(i1 == 0), stop=(i1 == 127)), reads=[B_HID[hs], B_VBUF[s_]], writes=[PB[ob]], sig=(tt == 1 and h2 == 1))

        emit_act(0)
        for pr in range(64):
            if pr + 1 < 64:
                emit_act(pr + 1)
            hph = pr // 32
            X, half = (blk, 1) if hph == 0 else (blk + 1, 0)
            j = pr % 32
            if (j % 2 == 1 or j == 0) and pend[0] is not None:
                build_pe(*pend[0])
                pend[0] = None
            emit_out(pr)
            if X < NB:
                if j % 2 == 0:
                    build_ra(X, half, j // 2)
                else:
                    build_lb(X, half, j // 2)
            if j % 2 == 1 and X < NB:
                pend[0] = (X, half, j // 2)
        if blk == NB - 1 and pend[0] is not None:
            build_pe(*pend[0])
            pend[0] = None

        for tt in range(2):
            i = blk * 2 + tt
            s_ = i % 2
            P.op("sp", lambda e, i=i, s_=s_: e.dma_start(out=x2t[s_], in_=x1s_d.ap()[i * 128:(i + 1) * 128, :]), reads=[B_X1S[i]], writes=[B_X2[s_]],
                 dma=True, semkey=("x2t", s_))
            for h2 in range(2):
                ob = 4 + tt * 2 + h2
                P.op("dve", lambda e, s_=s_, h2=h2, ob=ob: e.tensor_tensor(out=x2t[s_][:, h2 * 512:(h2 + 1) * 512], in0=x2t[s_][:, h2 * 512:(h2 + 1) * 512], in1=bank(ob), op=ALU.add),
                     reads=[PB[ob]], writes=[B_X2[s_]])
            if "x2" in dbg_d:
                dbg_out("x2", x2t[s_], [B_X2[s_]], i * 128, 128, 0, D)
            rms_tile(i, x2t[s_], gfb, s_, [B_X2[s_]], B_YT, yt)
            o = P.op("sp", lambda e, i=i: e.dma_start(out=out_d.ap()[i * 128:(i + 1) * 128, :], in_=yt), reads=[B_YT], writes=[B_OUT[i]], dma=True, semkey=("yt",))
            finals.append(o)

    P.emit(finals)
    return nc, es


def _host_consts(inputs):
    c = {}
    c["identb"] = np.eye(128, dtype=np.float32).astype(ml_dtypes.bfloat16)
    c["identf"] = np.eye(128, dtype=np.float32)
    c["iotab"] = np.tile(np.arange(128, dtype=np.float32)[None, :], (128, 1)).astype(ml_dtypes.bfloat16)
    c["iota16"] = np.tile(np.arange(16, dtype=np.float32)[None, :], (128, 1))
    rpb = np.asarray(inputs["na_rpb"], np.float32)[0]
    kc = np.arange(64)[:, None]
    cq = np.arange(64)[None, :]
    dci = np.clip(kc - cq, -15, 15) + 15
    g = rpb[:, :, dci]
    lo = g[:, 0:14].transpose(2, 1, 0, 3)
    hi = g[:, 1:15].transpose(2, 1, 0, 3)
    c["rbg"] = np.ascontiguousarray(np.concatenate([lo, hi], axis=0).reshape(128, 8 * 14 * 64), np.float32)
    cs = np.clip(np.arange(64) - 8, 0, 48)
    inwin = (kc >= cs[None, :]) & (kc < cs[None, :] + 16)
    nm = np.where(inwin, 0.0, NEG).astype(np.float32)
    c["negmask"] = np.ascontiguousarray(np.concatenate([nm, nm], axis=0))
    c["gq"] = np.ascontiguousarray(np.tile(np.asarray(inputs["gqa_q_norm_g"], np.float32)[0], 2).reshape(128, 1))
    c["gk"] = np.ascontiguousarray(np.tile(np.asarray(inputs["gqa_k_norm_g"], np.float32)[0], 2).reshape(128, 1))
    t = np.arange(S)
    inv = (10000.0 ** (-np.arange(16, dtype=np.float32) / 16)).astype(np.float32)
    ang_r = (t // 64).astype(np.float32)[None, :] * inv[:, None]
    ang_c = (t % 64).astype(np.float32)[None, :] * inv[:, None]
    ang = np.concatenate([ang_r, ang_r, ang_c, ang_c], axis=0)
    ang = np.concatenate([ang, ang], axis=0).astype(np.float32)
    c["rope_c"] = np.cos(ang).astype(np.float32)
    c["rope_s"] = np.sin(ang).astype(np.float32)
    R = np.zeros((128, 128), np.float32)
    for m in range(128):
        if m % 32 < 16:
            R[m, m + 16] = -1.0
        else:
            R[m, m - 16] = 1.0
    c["permT"] = np.ascontiguousarray(R.T)
    blk = np.zeros((128, 128), np.float32)
    blk[:64, :64] = 1.0 / 64
    blk[64:, 64:] = 1.0 / 64
    c["blk64"] = blk
    return c


def _in_maps(inputs, consts):
    shared = {
        "norm1_g": np.asarray(inputs["norm1_g"], np.float32).reshape(1, D),
        "w_in": np.asarray(inputs["w_in"], np.float32).reshape(D, 4352),
        "w_proj_a": np.asarray(inputs["w_proj_a"], np.float32).reshape(512, D),
        "w_proj_b": np.asarray(inputs["w_proj_b"], np.float32).reshape(512, D),
        "w_out": np.asarray(inputs["w_out"], np.float32).reshape(D, D),
        "norm2_g": np.asarray(inputs["norm2_g"], np.float32).reshape(1, D),
        "peer_w_q": np.asarray(inputs["peer_w_q"], np.float32).reshape(D, 2048),
        "peer_sub_keys": np.asarray(inputs["peer_sub_keys"], np.float32).reshape(16 * 128, 128),
        "peer_u": np.asarray(inputs["peer_u"], np.float32).reshape(16384, D),
        "peer_v": np.asarray(inputs["peer_v"], np.float32).reshape(16384, D),
        "norm_f_g": np.asarray(inputs["norm_f_g"], np.float32).reshape(1, D),
    }
    shared.update(consts)
    x = np.asarray(inputs["x"], np.float32)
    return [dict(shared, x=np.ascontiguousarray(x[b])) for b in range(8)]


def kernel(**inputs):
    nc, es = build()
    with es:
        pass
    maps = _in_maps(inputs, _host_consts(inputs))
    res = run_bass_kernel_spmd(nc, maps, core_ids=list(range(8)))
    return np.stack([r["out"] for r in res.results], axis=0).astype(np.float32)
```

```python
import contextlib
import numpy as np
import ml_dtypes
import concourse.bass as bass
import concourse.mybir as mybir
from concourse.bass_utils import run_bass_kernel_spmd

F32 = mybir.dt.float32
BF16 = mybir.dt.bfloat16
U32 = mybir.dt.uint32
ALU = mybir.AluOpType
AF = mybir.ActivationFunctionType

S = 2048
D = 1024
NT = 16
EPS = 1e-6
NEG = -30000.0


class Buf:
    __slots__ = ("name", "last_w", "readers", "writers")

    def __init__(self, name):
        self.name = name
        self.last_w = None
        self.readers = []
        self.writers = []


class Prog:
    ENGS = ("pe", "dve", "act", "pool", "sp")

    def __init__(self, nc, es):
        self.nc = nc
        self.es = es
        self.ops = []
        self.sem = {}
        self.cnt = {}
        self.opsig = []
        self.last_eng = {}
        self.last_dma = {}
        self.pending = {}
        self.sigflag = []

    def _sem(self, key):
        if key not in self.sem:
            self.sem[key] = self.es.enter_context(self.nc.semaphore("s%d" % len(self.sem)))
            self.cnt[key] = 0
        return self.sem[key]

    def _compact(self, lst):
        if len(lst) <= 48:
            return lst
        best = {}
        keep = []
        for o in lst:
            sg = self.opsig[o]
            if sg is None:
                keep.append(o)
            elif sg[0] not in best or self.opsig[best[sg[0]]][1] < sg[1]:
                best[sg[0]] = o
        return keep + list(best.values())

    def op(self, eng, fn, reads=(), writes=(), dma=False, semkey=None, extra_deps=(), sig=True, wdis=()):
        deps = set(extra_deps)
        for b in reads:
            if b.last_w is not None:
                deps.add(b.last_w)
            deps.update(b.writers)
        for b in writes:
            if b.last_w is not None:
                deps.add(b.last_w)
            deps.update(b.writers)
            deps.update(b.readers)
        for b in wdis:
            if b.last_w is not None:
                deps.add(b.last_w)
            deps.update(b.readers)
        oid = len(self.ops)
        if dma:
            key = ("dma", semkey)
            inc = 16
            self.last_dma[key] = oid
        else:
            key = ("eng", eng)
            inc = 1
            self.last_eng[eng] = oid
        self._sem(key)
        if sig:
            self.cnt[key] += inc
            self.opsig.append((key, self.cnt[key]))
            for po in self.pending.pop(key, []):
                self.opsig[po] = (key, self.cnt[key])
        else:
            assert not dma
            self.opsig.append(None)
            self.pending.setdefault(key, []).append(oid)
        self.sigflag.append(sig)
        self.ops.append((eng, fn, sorted(deps), dma))
        for b in reads:
            b.readers.append(oid)
            b.readers = self._compact(b.readers)
        for b in writes:
            b.last_w = oid
            b.readers = []
            b.writers = []
        for b in wdis:
            b.writers.append(oid)
            b.writers = self._compact(b.writers)
        return oid

    def barrier(self):
        deps = list(self.last_eng.values()) + list(self.last_dma.values())
        for eng in self.ENGS:
            self.op(eng, None, extra_deps=deps)

    def emit(self, final_wait_ops=()):
        nc = self.nc
        assert not any(self.pending.values()), "unsignalled trailing ops"
        block = self.es.enter_context(nc.Block())
        per_eng = {e: [] for e in self.ENGS}
        for oid, (eng, fn, deps, dma) in enumerate(self.ops):
            per_eng[eng].append(oid)
        prog = self

        def make(engname):
            def body(e):
                waited = {}
                for oid in per_eng[engname]:
                    eng, fn, deps, dma = prog.ops[oid]
                    need = {}
                    for d in deps:
                        deng, _, _, ddma = prog.ops[d]
                        if (not ddma) and deng == engname and engname == "pe":
                            continue
                        k, v = prog.opsig[d]
                        if need.get(k, 0) < v:
                            need[k] = v
                    for k, v in need.items():
                        if waited.get(k, 0) < v:
                            e.wait_ge(prog.sem[k], v)
                            waited[k] = v
                    k, v = prog.opsig[oid]
                    if fn is None:
                        e.sem_inc(prog.sem[k], 1)
                    else:
                        ins = fn(e)
                        if prog.sigflag[oid]:
                            ins.then_inc(prog.sem[k], 16 if dma else 1)
                if engname == "sp":
                    need = {}
                    for d in final_wait_ops:
                        k, v = prog.opsig[d]
                        if need.get(k, 0) < v:
                            need[k] = v
                    for k, v in need.items():
                        e.wait_ge(prog.sem[k], v)
            return body

        block.tensor(make("pe"))
        block.vector(make("dve"))
        block.scalar(make("act"))
        block.gpsimd(make("pool"))
        block.sync(make("sp"))


def _dtsize(dt):
    return 2 if dt == BF16 else 4


class Arena:
    def __init__(self, ar, nbytes):
        self.ar = ar
        self.nbytes = nbytes
        self.top = 0

    def alloc(self, shape, dt):
        n = int(np.prod(shape))
        nb = n * _dtsize(dt)
        off = self.top
        self.top += (nb + 63) // 64 * 64
        assert self.top <= self.nbytes, ("arena overflow", self.top, self.nbytes)
        v = self.ar[:, off // 2:(off + nb) // 2]
        if dt != BF16:
            v = v.bitcast(dt)
        if len(shape) == 2:
            v = v.rearrange("p (a b) -> p a b", a=shape[0])
        elif len(shape) == 3:
            v = v.rearrange("p (a b c) -> p a b c", a=shape[0], b=shape[1])
        elif len(shape) == 4:
            v = v.rearrange("p (a b c d) -> p a b c d", a=shape[0], b=shape[1], c=shape[2])
        return v


def cap(view, rel, dims, parts=None, pstart=0):
    ps = view.ap[0][0]
    npart = parts if parts is not None else view.ap[0][1]
    return bass.AP(tensor=view.tensor, offset=view.offset + pstart * ps + rel,
                   ap=[[ps, npart]] + [list(d) for d in dims])


def dap(t, off, dims):
    return bass.AP(tensor=t, offset=off, ap=[list(d) for d in dims])


ARENA_BYTES = 207 * 1024


NA_SUB = 9
NA_ROWS = 32
PK_TILES = 1


def build(stage=99, dbg=()):
    nc = bass.Bass("TRN2", target_bir_lowering=False)
    es = contextlib.ExitStack()

    def din(name, shape, dt=F32):
        return nc.dram_tensor(name, list(shape), dt, kind="ExternalInput")

    x_d = din("x", [S, D])
    g1_d = din("norm1_g", [1, D])
    win_d = din("w_in", [D, 4352])
    rb_d = din("rbg", [128, 8 * 14 * 64])
    nm_d = din("negmask", [128, 64])
    gq_d = din("gq", [128, 1])
    gk_d = din("gk", [128, 1])
    wpa_d = din("w_proj_a", [512, D])
    wpb_d = din("w_proj_b", [512, D])
    wo_d = din("w_out", [D, D])
    g2_d = din("norm2_g", [1, D])
    wq_d = din("peer_w_q", [D, 2048])
    sk_d = din("peer_sub_keys", [16 * 128, 128])
    u_d = din("peer_u", [16384, D])
    v_d = din("peer_v", [16384, D])
    gf_d = din("norm_f_g", [1, D])
    idb_d = din("identb", [128, 128], BF16)
    idf_d = din("identf", [128, 128])
    iob_d = din("iotab", [128, 128], BF16)
    io16_d = din("iota16", [128, 16])
    ropec_d = din("rope_c", [128, S])
    ropes_d = din("rope_s", [128, S])
    perm_d = din("permT", [128, 128])
    blk_d = din("blk64", [128, 128])
    out_d = nc.dram_tensor("out", [S, D], F32, kind="ExternalOutput")
    x1s_d = nc.dram_tensor("x1s", [S, D], F32, kind="Internal")
    vs_d = nc.dram_tensor("vs", [16384, D], BF16, kind="Internal")
    uts_d = nc.dram_tensor("uts", [128, 128 * 1024], BF16, kind="Internal")
    dbg_d = {}
    for name, shape in dbg:
        dbg_d[name] = nc.dram_tensor("dbg_" + name, list(shape), F32, kind="ExternalOutput")

    ar = es.enter_context(nc.sbuf_tensor("arena", [128, ARENA_BYTES // 2], BF16))
    ps = es.enter_context(nc.psum_tensor("ps", [128, 4096], F32))
    A = Arena(ar, ARENA_BYTES)
    psA = ps[:, :]
    P = Prog(nc, es)
    PB = [Buf("bank%d" % i) for i in range(8)]

    def bank(i, a=0, b=512):
        return ps[:, i * 512 + a:i * 512 + b]

    def bankb(i):
        return ps[:, i * 512:(i + 1) * 512].bitcast(BF16)

    _dq = [0]

    def dma_q():
        _dq[0] += 1
        return "sp"

    identb = A.alloc([128], BF16)
    identf = A.alloc([128], F32)
    iotab = A.alloc([128], BF16)
    iota16 = A.alloc([16], F32)
    B_C = Buf("consts")

    def ld(dst, src, q="sp", key=None):
        P.op(q, lambda e: e.dma_start(out=dst, in_=src), writes=[B_C], dma=True, semkey=("c", key or id(dst)))

    ld(identb, idb_d.ap(), key=0)
    ld(identf, idf_d.ap(), key=1)
    ld(iotab, iob_d.ap(), key=2)
    ld(iota16, io16_d.ap(), key=3)
    stats = A.alloc([64], F32)
    B_ST = [Buf("st%d" % i) for i in range(NT)]
    base_mark = A.top

    hT = A.alloc([8, S], BF16)
    B_HT = [Buf("hT%d" % i) for i in range(4)]
    wbf_mark = A.top
    wbf = [A.alloc([8, 512], BF16) for _ in range(2)]
    B_WB = [Buf("wb0"), Buf("wb1")]
    att_mark = A.top

    def dump(name, view_fn, reads, rows, cols):
        pass

    g1b = A.alloc([D], F32)
    ld(g1b, dap(g1_d, 0, [[0, 128], [1, D]]), key=4)
    xt = [A.alloc([D], F32) for _ in range(2)]
    hb = [A.alloc([D], BF16) for _ in range(2)]
    junk_ref = [A.alloc([D], BF16)]
    B_XT = [Buf("xt0"), Buf("xt1")]
    B_HB = [Buf("hb0"), Buf("hb1")]
    B_JK = Buf("junk")
    ss, rt, rstd = stats[:, 0:16], stats[:, 16:32], stats[:, 32:48]

    def rms_tile(i, src_view, gvec, s, reads_src, B_hb_s, hb_s):
        jk = junk_ref[0]
        P.op("act", lambda e: e.activation(out=jk, in_=src_view, func=AF.Square, accum_out=ss[:, i:i + 1]),
             reads=reads_src, writes=[B_JK, B_ST[i]])
        P.op("act", lambda e: e.activation(out=rt[:, i:i + 1], in_=ss[:, i:i + 1], func=AF.Sqrt, scale=1.0 / D, bias=EPS),
             reads=[B_ST[i]], writes=[B_ST[i]])
        P.op("dve", lambda e: e.reciprocal(out=rstd[:, i:i + 1], in_=rt[:, i:i + 1]), reads=[B_ST[i]], writes=[B_ST[i]])
        P.op("dve", lambda e: e.scalar_tensor_tensor(out=hb_s, in0=src_view, scalar=rstd[:, i:i + 1], in1=gvec,
                                                     op0=ALU.mult, op1=ALU.mult),
             reads=reads_src + [B_ST[i], B_C], writes=[B_hb_s])

    def transpose_tile_to(i, hb_s, B_hb_s, dstT, B_dst, bk):
        pb = bankb(bk)
        for k in range(8):
            P.op("pe", lambda e, k=k: e.transpose(pb[:, k * 128:(k + 1) * 128], hb_s[:, k * 128:(k + 1) * 128], identb),
                 reads=[B_hb_s, B_C], writes=[PB[bk]], sig=(k == 7))
        P.op("act", lambda e: e.activation(out=dstT[:, :, i * 128:(i + 1) * 128],
                                           in_=pb.rearrange("p (k t) -> p k t", k=8), func=AF.Copy),
             reads=[PB[bk]], writes=[B_dst])

    for i in range(NT):
        s = i % 2
        P.op("sp", lambda e, i=i, s=s: e.dma_start(out=xt[s], in_=x_d.ap()[i * 128:(i + 1) * 128, :]),
             writes=[B_XT[s]], dma=True, semkey=("xt", s))
        rms_tile(i, xt[s], g1b, s, [B_XT[s]], B_HB[s], hb[s])
        transpose_tile_to(i, hb[s], B_HB[s], hT, B_HT[i // 4], 6 + s)

    finals = []
    B_DBG = Buf("dbg")

    def dbg_out(name, sb_view, reads, r0, nrows, c0, ncols):
        if name in dbg_d:
            o = P.op("sp", lambda e: e.dma_start(out=dbg_d[name].ap()[r0:r0 + nrows, c0:c0 + ncols], in_=sb_view),
                     reads=reads, writes=[B_DBG], dma=True, semkey=("dbg",))
            finals.append(o)

    if "hT" in dbg_d:
        tmpf = A.alloc([S], F32)
        B_T = Buf("tmpf")
        for k in range(8):
            P.op("dve", lambda e, k=k: e.tensor_copy(out=tmpf, in_=hT[:, k, :]), reads=B_HT, writes=[B_T])
            dbg_out("hT", tmpf, [B_T], k * 128, 128, 0, S)

    if stage <= 0:
        P.emit(finals)
        return nc, es

    P.barrier()
    A.top = att_mark
    yaT = A.alloc([4, S], BF16)
    B_YAT, B_YBT = Buf("yaT"), Buf("ybT")
    if dbg_d:
        P.op("pool", lambda e: e.memset(yaT.rearrange("p a b -> p (a b)"), 0.0), writes=[B_YAT])
    ph_mark = A.top
    QaT = A.alloc([4, S], BF16)
    KaT = A.alloc([8, S], BF16)
    Va = A.alloc([16, 8, 65], BF16)
    Vas = A.alloc([15, 8, 65], BF16)
    RB2 = A.alloc([14, 8, 64], F32)
    negm = A.alloc([64], F32)
    B_QA, B_KA, B_VA, B_VAS, B_RB = Buf("QaT"), Buf("KaT"), Buf("Va"), Buf("Vas"), Buf("RB2")

    P.op("sp", lambda e: e.dma_start(out=RB2.rearrange("p a b c -> p (a b c)"), in_=rb_d.ap()), writes=[B_RB], dma=True, semkey=("c", 10))
    P.op("sp", lambda e: e.dma_start(out=negm, in_=nm_d.ap()), writes=[B_RB], dma=True, semkey=("c", 11))
    P.op("pool", lambda e: e.tensor_tensor(out=RB2.rearrange("p a b c -> p (a b) c"), in0=RB2.rearrange("p a b c -> p (a b) c"),
                                           in1=cap(negm, 0, [[0, 112], [1, 64]]), op=ALU.add), reads=[B_RB], writes=[B_RB])
    P.op("pool", lambda e: e.memset(KaT.rearrange("p a b -> p (a b)"), 0.0), writes=[B_KA])
    P.op("pool", lambda e: e.memset(Va.rearrange("p a b c -> p (a b c)"), 1.0), writes=[B_VA])
    P.op("pool", lambda e: e.memset(Vas.rearrange("p a b c -> p (a b c)"), 1.0), writes=[B_VAS])

    def load_w(slot, src_t, row_stride, c0, ncols, kch=8, dcol=0):
        dst = wbf[slot][:, 0:kch, dcol:dcol + ncols]
        P.op("pool", lambda e: e.dma_start(out=dst, in_=dap(src_t, c0, [[row_stride, 128], [128 * row_stride, kch], [1, ncols]])),
             writes=[B_WB[slot]], dma=True, semkey=("wb", slot))

    _bk = [0]

    def next_bank(n=6):
        b = _bk[0] % n
        _bk[0] += 1
        return b

    _ev = [0]

    def evac(out_view, in_view, reads, writes, eng=None):
        _ev[0] += 1
        if eng == "act" or (eng is None and _ev[0] % 2 == 0):
            P.op("act", lambda e: e.activation(out=out_view, in_=in_view, func=AF.Copy), reads=reads, wdis=writes)
        else:
            P.op("dve", lambda e: e.tensor_copy(out=out_view, in_=in_view), reads=reads, wdis=writes)

    def proj_fm(slot, ncols, dstT, B_dst, post=None):
        for c in range(ncols // 128):
            for tc in range(4):
                b = next_bank()
                for k in range(8):
                    P.op("pe", lambda e, k=k, c=c, tc=tc, b=b: e.matmul(bank(b), lhsT=wbf[slot][:, k, c * 128:(c + 1) * 128],
                                                                        rhs=hT[:, k, tc * 512:(tc + 1) * 512], start=(k == 0), stop=(k == 7)),
                         reads=[B_WB[slot], B_HT[tc]], writes=[PB[b]], sig=(k == 7))
                if post is None:
                    evac(dstT[:, c, tc * 512:(tc + 1) * 512], bank(b), [PB[b]], [B_dst])
                else:
                    post(c, tc, b)

    def proj_tm(slot, ncols, tok_off, ntiles, dst_fn, B_dst):
        for i in range(ntiles):
            b = next_bank()
            t0 = tok_off + i * 128
            for k in range(8):
                P.op("pe", lambda e, k=k, b=b, t0=t0: e.matmul(bank(b, 0, ncols), lhsT=hT[:, k, t0:t0 + 128],
                                                               rhs=wbf[slot][:, k, 0:ncols], start=(k == 0), stop=(k == 7)),
                     reads=[B_WB[slot]] + B_HT, writes=[PB[b]], sig=(k == 7))
            evac(dst_fn(i), bank(b, 0, ncols).rearrange("p (h d) -> p h d", d=64), [PB[b]], [B_dst])

    load_w(0, win_d, 4352, 0, 512)
    load_w(1, win_d, 4352, 512, 512)
    proj_fm(0, 512, QaT, B_QA)
    load_w(0, win_d, 4352, 1024, 512)
    def ka_post(c, tc, b):
        evac(KaT[0:64, 2 * c, tc * 512:(tc + 1) * 512], ps[0:64, b * 512:(b + 1) * 512], [PB[b]], [B_KA])
        evac(KaT[64:128, 2 * c + 1, tc * 512:(tc + 1) * 512], ps[64:128, b * 512:(b + 1) * 512], [PB[b]], [B_KA])

    proj_fm(1, 512, KaT, B_KA, post=ka_post)
    proj_tm(0, 512, 0, 16, lambda i: Va[:, i, :, 0:64], B_VA)
    proj_tm(0, 512, 64, 15, lambda i: Vas[:, i, :, 0:64], B_VAS)
    load_w(1, win_d, 4352, 1536, 512)

    if stage <= 0.5:
        tmpf = A.alloc([S], F32)
        B_T = Buf("tmpf2")
        if "QaT" in dbg_d:
            for k in range(4):
                P.op("dve", lambda e, k=k: e.tensor_copy(out=tmpf, in_=QaT[:, k, :]), reads=[B_QA], writes=[B_T])
                dbg_out("QaT", tmpf, [B_T], k * 128, 128, 0, S)
        if "Va" in dbg_d:
            for i in range(16):
                P.op("dve", lambda e, i=i: e.tensor_copy(out=tmpf[:, 0:512].rearrange("p (h d) -> p h d", d=64), in_=Va[:, i, :, 0:64]), reads=[B_VA], writes=[B_T])
                dbg_out("Va", tmpf[:, 0:512], [B_T], i * 128, 128, 0, 512)
        P.emit(finals)
        return nc, es
    ssb = [A.alloc([1024], F32) for _ in range(2)]
    PT = [A.alloc([1024], BF16) for _ in range(2)]
    yar = [A.alloc([512], BF16) for _ in range(2)]
    rec = A.alloc([2, 4], F32)
    B_SSB = [Buf("ssb0"), Buf("ssb1")]
    B_PT = [Buf("pt0"), Buf("pt1")]
    B_YAR = [Buf("yar0"), Buf("yar1")]
    B_REC = [Buf("rec0"), Buf("rec1")]
    na_steps = [(r, g) for r in range(32) for g in range(2)]

    def na_A(step):
        r, g = na_steps[step]
        rs = min(max(r - 4, 0), 24)
        ks = rs * 64
        dr0 = rs - r + 7
        sl = step % 2
        sb0 = sl * 2
        for hh in range(4):
            h = g * 4 + hh
            for kt in range(4):
                o0 = sb0 * 512 + kt * 256 + hh * 64
                bkw = sb0 + (kt // 2)
                P.op("pe", lambda e, o0=o0, h=h, kt=kt, ks=ks, r=r: e.matmul(
                    ps[:, o0:o0 + 64], lhsT=KaT[:, h, ks + kt * 128:ks + (kt + 1) * 128],
                    rhs=QaT[:, h // 2, r * 64:(r + 1) * 64], start=True, stop=True),
                    reads=[B_KA, B_QA], writes=[PB[bkw]], sig=(hh == 3 and kt == 3))
        P.op("dve", lambda e, sl=sl, sb0=sb0, g=g, dr0=dr0: e.scalar_tensor_tensor(
            out=ssb[sl].rearrange("p (k q) -> p k q", k=4),
            in0=ps[:, sb0 * 512:sb0 * 512 + 1024].rearrange("p (k q) -> p k q", k=4),
            scalar=0.125, in1=cap(RB2, (dr0 * 8 + g * 4) * 64, [[1024, 4], [1, 256]]),
            op0=ALU.mult, op1=ALU.add),
            reads=[PB[sb0], PB[sb0 + 1], B_RB], writes=[B_SSB[sl]])
        P.op("act", lambda e, sl=sl: e.activation(out=PT[sl], in_=ssb[sl], func=AF.Exp), reads=[B_SSB[sl]], writes=[B_PT[sl]])

    def na_B(step):
        r, g = na_steps[step]
        rs = min(max(r - 4, 0), 24)
        sl = step % 2
        ys = r % 2
        pvb = 4 + sl
        for hh in range(4):
            h = g * 4 + hh
            for kt in range(4):
                if rs % 2 == 0:
                    vsrc, bv, ti = Va, B_VA, rs // 2 + kt
                else:
                    vsrc, bv, ti = Vas, B_VAS, (rs - 1) // 2 + kt
                P.op("pe", lambda e, pvb=pvb, hh=hh, h=h, kt=kt, sl=sl, vsrc=vsrc, ti=ti: e.matmul(
                    ps[0:64, pvb * 512 + hh * 65:pvb * 512 + (hh + 1) * 65],
                    lhsT=PT[sl][:, kt * 256 + hh * 64:kt * 256 + (hh + 1) * 64], rhs=vsrc[:, ti, h, :],
                    start=(kt == 0), stop=(kt == 3)),
                    reads=[B_PT[sl], bv], writes=[PB[pvb]], sig=(hh == 3 and kt == 3))
        P.op("dve", lambda e, pvb=pvb, sl=sl: e.reciprocal(out=rec[0:64, sl, :], in_=cap(psA, pvb * 512 + 64, [[65, 4]], parts=64)),
             reads=[PB[pvb]], writes=[B_REC[sl]])
        P.op("dve", lambda e, pvb=pvb, sl=sl, g=g, ys=ys: e.tensor_tensor(
            out=yar[ys][0:64, g * 256:(g + 1) * 256].rearrange("p (h d) -> p h d", h=4),
            in0=cap(psA, pvb * 512, [[65, 4], [1, 64]], parts=64),
            in1=cap(rec, sl * 4, [[1, 4], [0, 64]], parts=64), op=ALU.mult),
            reads=[PB[pvb], B_REC[sl]], wdis=[B_YAR[ys]])
        if g == 1:
            tb = 6 + ys
            pbt = bankb(tb)
            for c in range(4):
                P.op("pe", lambda e, c=c, ys=ys, pbt=pbt: e.transpose(pbt[:, c * 64:(c + 1) * 64], yar[ys][0:64, c * 128:(c + 1) * 128], identb[0:64, 0:64]),
                     reads=[B_YAR[ys], B_C], writes=[PB[tb]], sig=(c == 3))
            P.op("act", lambda e, r=r, pbt=pbt: e.activation(out=yaT[:, :, r * 64:(r + 1) * 64], in_=pbt[:, 0:256].rearrange("p (c t) -> p c t", c=4), func=AF.Copy),
                 reads=[PB[tb]], writes=[B_YAT])

    na_A(0)
    for step in range(64):
        if step + 1 < 64:
            na_A(step + 1)
        na_B(step)

    def dump_fm(name, srcT, B_src, nch):
        if name in dbg_d:
            tmpf = A.alloc([1024], F32)
            B_T = Buf("tmpf_" + name)
            for k in range(nch):
                for hf in range(2):
                    P.op("dve", lambda e, k=k, hf=hf: e.tensor_copy(out=tmpf, in_=srcT[:, k, hf * 1024:(hf + 1) * 1024]), reads=[B_src], writes=[B_T])
                    dbg_out(name, tmpf, [B_T], k * 128, 128, hf * 1024, 1024)

    dump_fm("yaT", yaT, B_YAT, 4)
    if stage <= 1:
        P.emit(finals)
        return nc, es

    P.barrier()
    A.top = ph_mark
    ybT = A.alloc([4, S], BF16)
    if dbg_d:
        P.op("pool", lambda e: e.memset(ybT.rearrange("p a b -> p (a b)"), 0.0), writes=[B_YBT])
    mg_mark = A.top
    QbT = A.alloc([4, S], BF16)
    KbT = A.alloc([4, S], BF16)
    Vb = A.alloc([16, 2, 65], BF16)
    ropeC = A.alloc([S], F32)
    ropeS = A.alloc([S], F32)
    permT = A.alloc([128], F32)
    blk64 = A.alloc([128], F32)
    gqv = A.alloc([1], F32)
    gkv = A.alloc([1], F32)
    B_QB, B_KB, B_VB, B_RC = Buf("QbT"), Buf("KbT"), Buf("Vb"), Buf("ropec")
    for dst, src, key in ((ropeC, ropec_d, 20), (ropeS, ropes_d, 21), (permT, perm_d, 22), (blk64, blk_d, 23), (gqv, gq_d, 24), (gkv, gk_d, 25)):
        P.op("sp", lambda e, dst=dst, src=src: e.dma_start(out=dst, in_=src.ap()), writes=[B_RC], dma=True, semkey=("c", key))
    P.op("pool", lambda e: e.memset(Vb.rearrange("p a b c -> p (a b c)"), 1.0), writes=[B_VB])
    P.op("pool", lambda e: e.memset(KbT.rearrange("p a b -> p (a b)"), 0.0), writes=[B_KB])
    sqf = A.alloc([512], F32)
    rtf = A.alloc([512], F32)
    rsf = A.alloc([512], F32)
    qn = A.alloc([512], F32)
    t1 = A.alloc([512], F32)
    t2 = A.alloc([512], F32)
    B_SQ, B_RT, B_RS, B_QN, B_T1, B_T2 = (Buf(n) for n in ("sqf", "rtf", "rsf", "qn", "t1", "t2"))

    def qk_post(dstT, B_dst, gvec, kmode=False):
        def post(c, tc, b):
            b2 = next_bank()
            P.op("act", lambda e: e.activation(out=sqf, in_=bank(b), func=AF.Square), reads=[PB[b]], writes=[B_SQ])
            P.op("pe", lambda e: e.matmul(bank(b2), lhsT=blk64, rhs=sqf, start=True, stop=True), reads=[B_SQ, B_RC], writes=[PB[b2]])
            P.op("act", lambda e: e.activation(out=rtf, in_=bank(b2), func=AF.Sqrt, bias=EPS), reads=[PB[b2]], writes=[B_RT])
            P.op("dve", lambda e: e.reciprocal(out=rsf, in_=rtf), reads=[B_RT], writes=[B_RS])
            P.op("dve", lambda e: e.scalar_tensor_tensor(out=qn, in0=bank(b), scalar=gvec[:, 0:1], in1=rsf, op0=ALU.mult, op1=ALU.mult),
                 reads=[PB[b], B_RS, B_RC], writes=[B_QN])
            b3 = next_bank()
            P.op("pe", lambda e: e.matmul(bank(b3), lhsT=permT, rhs=qn, start=True, stop=True), reads=[B_QN, B_RC], writes=[PB[b3]])
            P.op("dve", lambda e: e.tensor_tensor(out=t1, in0=qn, in1=ropeC[:, tc * 512:(tc + 1) * 512], op=ALU.mult), reads=[B_QN, B_RC], writes=[B_T1])
            P.op("dve", lambda e: e.tensor_tensor(out=t2, in0=bank(b3), in1=ropeS[:, tc * 512:(tc + 1) * 512], op=ALU.mult), reads=[PB[b3], B_RC], writes=[B_T2])
            if not kmode:
                P.op("pool", lambda e: e.tensor_tensor(out=dstT[:, c, tc * 512:(tc + 1) * 512], in0=t1, in1=t2, op=ALU.add), reads=[B_T1, B_T2], writes=[B_dst])
            else:
                lo_idx = 0 if c == 0 else 2
                hi_idx = 3 if c == 0 else 1
                P.op("pool", lambda e: e.tensor_tensor(out=dstT[0:64, lo_idx, tc * 512:(tc + 1) * 512], in0=t1[0:64, :], in1=t2[0:64, :], op=ALU.add), reads=[B_T1, B_T2], writes=[B_dst])
                P.op("pool", lambda e: e.tensor_tensor(out=dstT[64:128, hi_idx, tc * 512:(tc + 1) * 512], in0=t1[64:128, :], in1=t2[64:128, :], op=ALU.add), reads=[B_T1, B_T2], writes=[B_dst])
        return post

    load_w(0, win_d, 4352, 2048, 128, dcol=0)
    load_w(0, win_d, 4352, 2112, 64, dcol=128)
    load_w(0, win_d, 4352, 2048, 64, dcol=192)
    load_w(0, win_d, 4352, 2176, 128, dcol=256)
    proj_fm(1, 512, QbT, B_QB, post=qk_post(QbT, B_QB, gqv))
    proj_fm(0, 256, KbT, B_KB, post=qk_post(KbT, B_KB, gkv, kmode=True))
    for i in range(16):
        b = next_bank()
        for k in range(8):
            P.op("pe", lambda e, k=k, b=b, i=i: e.matmul(bank(b, 0, 128), lhsT=hT[:, k, i * 128:(i + 1) * 128], rhs=wbf[0][:, k, 256:384],
                                                      start=(k == 0), stop=(k == 7)), reads=[B_WB[0]] + B_HT, writes=[PB[b]], sig=(k == 7))
        evac(Vb[:, i, :, 0:64], bank(b, 0, 128).rearrange("p (h d) -> p h d", d=64), [PB[b]], [B_VB])

    dump_fm("QbT", QbT, B_QB, 4)
    dump_fm("KbT", KbT, B_KB, 4)

    PTf = [A.alloc([16, 512], BF16) for _ in range(2)]
    B_PTF = [Buf("ptf0"), Buf("ptf1")]
    ybt = A.alloc([4, 512], BF16)
    B_YBTOK = Buf("ybtok")
    recg = A.alloc([2, 4], F32)
    B_RECG = [Buf("recg0"), Buf("recg1")]
    _sg = [0]

    def emit_S(hc, kb2):
        qc, h = hc // 8, hc % 8
        sl = _sg[0] % 2
        _sg[0] += 1
        hs = hc % 2
        kvh = h // 4
        for j in range(2):
            kt = kb2 * 2 + j
            bk = sl * 2 + j
            P.op("pe", lambda e, bk=bk, kvh=kvh, kt=kt, h=h, qc=qc: e.matmul(
                bank(bk), lhsT=KbT[:, kvh * 2 + (h % 2), kt * 128:(kt + 1) * 128],
                rhs=QbT[:, h // 2, qc * 512:(qc + 1) * 512], start=True, stop=True),
                reads=[B_KB, B_QB], writes=[PB[bk]])
        P.op("act", lambda e, sl=sl, hs=hs, kb2=kb2: e.activation(out=PTf[hs][:, kb2 * 2:kb2 * 2 + 2, :].rearrange("p a b -> p (a b)"),
                                                            in_=ps[:, sl * 1024:(sl + 1) * 1024], func=AF.Exp, scale=0.125),
             reads=[PB[sl * 2], PB[sl * 2 + 1]], wdis=[B_PTF[hs]])

    def emit_PVall(hc):
        qc, h = hc // 8, hc % 8
        hs = hc % 2
        kvh = h // 4
        pvb = 4 + hs
        for qt in range(4):
            for kt in range(16):
                P.op("pe", lambda e, pvb=pvb, qt=qt, hs=hs, kt=kt, kvh=kvh: e.matmul(
                    ps[:, pvb * 512 + qt * 65:pvb * 512 + (qt + 1) * 65], lhsT=PTf[hs][:, kt, qt * 128:(qt + 1) * 128],
                    rhs=Vb[:, kt, kvh, :], start=(kt == 0), stop=(kt == 15)),
                    reads=[B_PTF[hs], B_VB], writes=[PB[pvb]], sig=(qt == 3 and kt == 15))
        P.op("dve", lambda e, pvb=pvb, hs=hs: e.reciprocal(out=recg[:, hs, :], in_=cap(psA, pvb * 512 + 64, [[65, 4]])),
             reads=[PB[pvb]], writes=[B_RECG[hs]])
        P.op("dve", lambda e, pvb=pvb, hs=hs, h=h: e.tensor_tensor(
            out=ybt[:, :, h * 64:(h + 1) * 64], in0=cap(psA, pvb * 512, [[65, 4], [1, 64]]),
            in1=cap(recg, hs * 4, [[1, 4], [0, 64]]), op=ALU.mult),
            reads=[PB[pvb], B_RECG[hs]], writes=[B_YBTOK])
        if h == 7:
            for qt in range(4):
                tb = 6 + qt % 2
                pbt = bankb(tb)
                for c in range(4):
                    P.op("pe", lambda e, c=c, qt=qt, pbt=pbt: e.transpose(pbt[:, c * 128:(c + 1) * 128], ybt[:, qt, c * 128:(c + 1) * 128], identb),
                         reads=[B_YBTOK, B_C], writes=[PB[tb]], sig=(c == 3))
                t0 = qc * 512 + qt * 128
                P.op("act", lambda e, t0=t0, pbt=pbt: e.activation(out=ybT[:, :, t0:t0 + 128], in_=pbt[:, 0:512].rearrange("p (c t) -> p c t", c=4), func=AF.Copy),
                     reads=[PB[tb]], writes=[B_YBT])

    for hc in range(33):
        if hc < 32:
            for kb2 in range(8):
                emit_S(hc, kb2)
                if kb2 == 1 and hc > 0:
                    emit_PVall(hc - 1)
        else:
            emit_PVall(31)

    dump_fm("ybT", ybT, B_YBT, 4)
    if stage <= 2:
        P.emit(finals)
        return nc, es


    P.barrier()
    A.top = mg_mark
    mT = A.alloc([8, S], BF16)
    B_MT = Buf("mT")
    md_mark = A.top
    wpa = A.alloc([4, D], BF16)
    wpb = A.alloc([4, D], BF16)
    B_WP = Buf("wp")
    P.op("pool", lambda e: e.dma_start(out=wpa, in_=dap(wpa_d, 0, [[D, 128], [128 * D, 4], [1, D]])), writes=[B_WP], dma=True, semkey=("c", 30))
    P.op("pool", lambda e: e.dma_start(out=wpb, in_=dap(wpb_d, 0, [[D, 128], [128 * D, 4], [1, D]])), writes=[B_WP], dma=True, semkey=("c", 31))
    sga = A.alloc([512], BF16)
    sgb = A.alloc([512], BF16)
    m1 = A.alloc([512], F32)
    m2 = A.alloc([512], F32)
    B_SGA, B_SGB, B_M1, B_M2 = Buf("sga"), Buf("sgb"), Buf("m1"), Buf("m2")
    for grp in range(2):
        load_w(0, win_d, 4352, 2304 + grp * 512, 512)
        load_w(1, win_d, 4352, 3328 + grp * 512, 512)
        for cn in range(4):
            n = grp * 4 + cn
            for tc in range(4):
                for (slot, wp, yT, B_y, sg, B_sg, mm, B_mm) in ((0, wpa, yaT, B_YAT, sga, B_SGA, m1, B_M1), (1, wpb, ybT, B_YBT, sgb, B_SGB, m2, B_M2)):
                    b = next_bank()
                    for k in range(8):
                        P.op("pe", lambda e, k=k, b=b, slot=slot, cn=cn, tc=tc: e.matmul(bank(b), lhsT=wbf[slot][:, k, cn * 128:(cn + 1) * 128],
                                                                                  rhs=hT[:, k, tc * 512:(tc + 1) * 512], start=(k == 0), stop=(k == 7)),
                             reads=[B_WB[slot], B_HT[tc]], writes=[PB[b]], sig=(k == 7))
                    P.op("act", lambda e, b=b, sg=sg: e.activation(out=sg, in_=bank(b), func=AF.Sigmoid), reads=[PB[b]], writes=[B_sg])
                    b2 = next_bank()
                    for c in range(4):
                        P.op("pe", lambda e, c=c, b2=b2, wp=wp, yT=yT, n=n, tc=tc: e.matmul(bank(b2), lhsT=wp[:, c, n * 128:(n + 1) * 128],
                                                                                      rhs=yT[:, c, tc * 512:(tc + 1) * 512], start=(c == 0), stop=(c == 3)),
                             reads=[B_WP, B_y], writes=[PB[b2]], sig=(c == 3))
                    P.op("dve", lambda e, b2=b2, sg=sg, mm=mm: e.tensor_tensor(out=mm, in0=sg, in1=bank(b2), op=ALU.mult), reads=[PB[b2], B_sg], writes=[B_mm])
                P.op("pool", lambda e, n=n, tc=tc: e.tensor_tensor(out=mT[:, n, tc * 512:(tc + 1) * 512], in0=m1, in1=m2, op=ALU.add),
                     reads=[B_M1, B_M2], writes=[B_MT])
    dump_fm("mT", mT, B_MT, 8)
    if stage <= 2.5:
        P.emit(finals)
        return nc, es

    P.barrier()
    A.top = md_mark
    xnT = hT
    B_XNT = B_HT
    wo = A.alloc([8, D], BF16)
    B_WO = Buf("wo")
    for kh in range(4):
        P.op("pool", lambda e, kh=kh: e.dma_start(out=wo[:, kh * 2:(kh + 1) * 2, :], in_=dap(wo_d, kh * 2 * 128 * D, [[D, 128], [128 * D, 2], [1, D]])),
             writes=[B_WO], dma=True, semkey=("c", 32))
    g2b = A.alloc([D], F32)
    P.op("sp", lambda e: e.dma_start(out=g2b, in_=dap(g2_d, 0, [[0, 128], [1, D]])), writes=[B_C], dma=True, semkey=("c", 33))
    xt2 = [A.alloc([D], F32) for _ in range(2)]
    x1t = [A.alloc([D], F32) for _ in range(2)]
    xnb = [A.alloc([D], BF16) for _ in range(2)]
    junk_ref[0] = A.alloc([D], BF16)
    B_XT2 = [Buf("xt2_0"), Buf("xt2_1")]
    B_X1T = [Buf("x1t0"), Buf("x1t1")]
    B_XNB = [Buf("xnb0"), Buf("xnb1")]
    B_X1S = [Buf("x1s%d" % i) for i in range(NT)]
    for i in range(NT):
        s_ = i % 2
        P.op("sp", lambda e, i=i, s_=s_: e.dma_start(out=xt2[s_], in_=x_d.ap()[i * 128:(i + 1) * 128, :]), writes=[B_XT2[s_]], dma=True, semkey=("xt2", s_))
        for half in range(2):
            b = next_bank()
            for k in range(8):
                P.op("pe", lambda e, k=k, b=b, i=i, half=half: e.matmul(bank(b), lhsT=mT[:, k, i * 128:(i + 1) * 128], rhs=wo[:, k, half * 512:(half + 1) * 512],
                                                                    start=(k == 0), stop=(k == 7)), reads=[B_MT, B_WO], writes=[PB[b]], sig=(k == 7))
            P.op("dve", lambda e, b=b, s_=s_, half=half: e.tensor_tensor(out=x1t[s_][:, half * 512:(half + 1) * 512], in0=xt2[s_][:, half * 512:(half + 1) * 512],
                                                                   in1=bank(b), op=ALU.add), reads=[PB[b], B_XT2[s_]], writes=[B_X1T[s_]])
        P.op("sp", lambda e, i=i, s_=s_: e.dma_start(out=x1s_d.ap()[i * 128:(i + 1) * 128, :], in_=x1t[s_]), reads=[B_X1T[s_]], writes=[B_X1S[i]],
             dma=True, semkey=("x1t", s_))
        rms_tile(i, x1t[s_], g2b, s_, [B_X1T[s_]], B_XNB[s_], xnb[s_])
        transpose_tile_to(i, xnb[s_], B_XNB[s_], xnT, B_XNT[i // 4], 6 + s_)
        if "x1" in dbg_d:
            dbg_out("x1", x1t[s_], [B_X1T[s_]], i * 128, 128, 0, D)
    if "xnT" in dbg_d:
        tmpf = A.alloc([1024], F32)
        B_T = Buf("tmpf_xn")
        for k in range(8):
            for hf in range(2):
                P.op("dve", lambda e, k=k, hf=hf: e.tensor_copy(out=tmpf, in_=xnT[:, k, hf * 1024:(hf + 1) * 1024]), reads=B_XNT, writes=[B_T])
                dbg_out("xnT", tmpf, [B_T], k * 128, 128, hf * 1024, 1024)
    if stage <= 3:
        P.emit(finals)
        return nc, es

    P.barrier()
    A.top = wbf_mark
    abgT = A.alloc([3, S], BF16)
    B_ABG = Buf("abgT")
    pk_mark = A.top
    A.top = att_mark
    wq = A.alloc([8, 2048], BF16)
    B_WQ = Buf("wq")
    for q4 in range(4):
        P.op("pool", lambda e, q4=q4: e.dma_start(out=wq[:, :, q4 * 512:(q4 + 1) * 512], in_=dap(wq_d, q4 * 512, [[2048, 128], [128 * 2048, 8], [1, 512]])),
             writes=[B_WQ], dma=True, semkey=("c", 40))
    skf = A.alloc([16, 128], F32)
    keysT = A.alloc([16, 128], BF16)
    B_SK, B_KT = Buf("skf"), Buf("keysT")
    P.op("sp", lambda e: e.dma_start(out=skf, in_=dap(sk_d, 0, [[128, 128], [128 * 128, 16], [1, 128]])), writes=[B_SK], dma=True, semkey=("c", 41))
    for hp in range(16):
        b = 4 + hp % 2
        P.op("pe", lambda e, hp=hp, b=b: e.transpose(bank(b, 0, 128), skf[:, hp, :], identf), reads=[B_SK, B_C], writes=[PB[b]])
        evac(keysT[:, hp, :], bank(b, 0, 128), [PB[b]], [B_KT])
    if stage <= 3.2:
        P.emit(finals)
        return nc, es
    qT = A.alloc([16, 512], BF16)
    B_QT = Buf("qT")
    sc = A.alloc([16, 128], F32)
    B_SC = Buf("sc")
    tv = A.alloc([16, 16], F32)
    tix = A.alloc([16, 16], U32)
    tixf = A.alloc([16, 16], F32)
    tmp1 = A.alloc([128], F32)
    cand = A.alloc([8, 256], F32)
    tmp2 = A.alloc([256], F32)
    bs = A.alloc([8, 16], F32)
    pos = A.alloc([8, 16], U32)
    k1u = A.alloc([128], U32)
    k2u = A.alloc([128], U32)
    k1f = A.alloc([128], F32)
    k2f = A.alloc([128], F32)
    eq1 = A.alloc([128, 16], F32)
    eq2 = A.alloc([128, 16], F32)
    abg = A.alloc([3, 128], F32)
    ee = A.alloc([8, 16], F32)
    zz = A.alloc([2, 8], F32)
    B_TK, B_PL, B_AB, B_EE = Buf("topk"), Buf("poolgather"), Buf("abg"), Buf("ee")

    ub = [A.alloc([D], BF16) for _ in range(2)]
    usb = [A.alloc([8, 128], BF16) for _ in range(2)]
    B_UB = [Buf("ub0"), Buf("ub1")]
    B_USB = [Buf("usb0"), Buf("usb1")]
    B_VS = [Buf("vs%d" % i) for i in range(32)]
    B_VSK = [Buf("vsk%d" % i) for i in range(4)]
    B_UTS = [Buf("uts%d" % i) for i in range(128)]

    def prepass_chunk(ec):
        if ec % 2 == 0:
            c2 = ec // 2
            P.op("pool", lambda e, c2=c2: e.dma_start(out=vs_d.ap()[c2 * 256:(c2 + 1) * 256, :], in_=v_d.ap()[c2 * 256:(c2 + 1) * 256, :]),
                 writes=[B_VS[c2 // 2], B_VSK[0]], dma=True, semkey=("vs", 0))
        s_ = ec % 2
        P.op("pool", lambda e, ec=ec, s_=s_: e.dma_start(out=ub[s_], in_=u_d.ap()[ec * 128:(ec + 1) * 128, :]), writes=[B_UB[s_]], dma=True, semkey=("ub", s_))
        b = 6 + s_
        pb = bankb(b)
        for k in range(8):
            P.op("pe", lambda e, k=k, s_=s_, pb=pb: e.transpose(pb[:, k * 128:(k + 1) * 128], ub[s_][:, k * 128:(k + 1) * 128], identb),
                 reads=[B_UB[s_], B_C], writes=[PB[b]], sig=(k == 7))
        evac(usb[s_].rearrange("p a b -> p (a b)"), pb, [PB[b]], [B_USB[s_]], eng="act")
        P.op("sp", lambda e, ec=ec, s_=s_: e.dma_start(out=dap(uts_d, ec * 131072, [[1024, 128], [1, 1024]]), in_=usb[s_].rearrange("p a b -> p (a b)")),
             reads=[B_USB[s_]], writes=[B_UTS[ec]], dma=True, semkey=("usb", s_))

    for tc in range(4):
        for hp in range(16):
            b = 4 + hp % 2
            for k in range(8):
                P.op("pe", lambda e, k=k, b=b, hp=hp, tc=tc: e.matmul(bank(b), lhsT=wq[:, k, hp * 128:(hp + 1) * 128], rhs=xnT[:, k, tc * 512:(tc + 1) * 512],
                                                                 start=(k == 0), stop=(k == 7)), reads=[B_WQ, B_XNT[tc]], writes=[PB[b]], sig=(k == 7))
            evac(qT[:, hp, :], bank(b), [PB[b]], [B_QT])
        for j in range(4):
            i = tc * 4 + j
            for hp in range(16):
                P.op("pe", lambda e, hp=hp, j=j: e.matmul(ps[:, hp * 128:(hp + 1) * 128], lhsT=qT[:, hp, j * 128:(j + 1) * 128], rhs=keysT[:, hp, :], start=True, stop=True),
                     reads=[B_QT, B_KT], writes=[PB[hp // 4]], sig=(hp == 15))
            P.op("act", lambda e: e.activation(out=sc.rearrange("p a b -> p (a b)"), in_=ps[:, 0:2048], func=AF.Copy), reads=PB[0:4], writes=[B_SC])
            if "sc" in dbg_d:
                dbg_out("sc", sc.rearrange("p a b -> p (a b)"), [B_SC], i * 128, 128, 0, 2048)
            if stage <= 3.4:
                P.emit(finals)
                return nc, es
            for cc in range(8):
                prepass_chunk(i * 8 + cc)
            for hp in range(16):
                P.op("dve", lambda e, hp=hp: e.max(out=tv[:, hp, 0:8], in_=sc[:, hp, :]), reads=[B_SC], writes=[B_TK])
                P.op("dve", lambda e, hp=hp: e.max_index(out=tix[:, hp, 0:8], in_max=tv[:, hp, 0:8], in_values=sc[:, hp, :]), reads=[B_SC, B_TK], writes=[B_TK])
                P.op("dve", lambda e, hp=hp: e.match_replace(out=tmp1, in_to_replace=tv[:, hp, 0:8], in_values=sc[:, hp, :], imm_value=-1e30), reads=[B_SC, B_TK], writes=[B_TK])
                P.op("dve", lambda e, hp=hp: e.max(out=tv[:, hp, 8:16], in_=tmp1), reads=[B_TK], writes=[B_TK])
                P.op("dve", lambda e, hp=hp: e.max_index(out=tix[:, hp, 8:16], in_max=tv[:, hp, 8:16], in_values=tmp1), reads=[B_TK], writes=[B_TK])
            if stage <= 3.5:
                P.emit(finals)
                return nc, es
            P.op("pool", lambda e: e.tensor_copy(out=tixf.rearrange("p a b -> p (a b)"), in_=tix.rearrange("p a b -> p (a b)")), reads=[B_TK], writes=[B_PL])
            if stage <= 3.6:
                P.emit(finals)
                return nc, es
            for h in range(8):
                P.op("dve", lambda e, h=h: e.tensor_tensor(out=cand[:, h, :].rearrange("p (a b) -> p a b", a=16),
                                                           in0=cap(tv, (2 * h) * 16, [[1, 16], [0, 16]]), in1=cap(tv, (2 * h + 1) * 16, [[0, 16], [1, 16]]), op=ALU.add),
                     reads=[B_TK], writes=[B_TK])
                P.op("dve", lambda e, h=h: e.max(out=bs[:, h, 0:8], in_=cand[:, h, :]), reads=[B_TK], writes=[B_TK])
                P.op("dve", lambda e, h=h: e.max_index(out=pos[:, h, 0:8], in_max=bs[:, h, 0:8], in_values=cand[:, h, :]), reads=[B_TK], writes=[B_TK])
                P.op("dve", lambda e, h=h: e.match_replace(out=tmp2, in_to_replace=bs[:, h, 0:8], in_values=cand[:, h, :], imm_value=-1e30), reads=[B_TK], writes=[B_TK])
                P.op("dve", lambda e, h=h: e.max(out=bs[:, h, 8:16], in_=tmp2), reads=[B_TK], writes=[B_TK])
                P.op("dve", lambda e, h=h: e.max_index(out=pos[:, h, 8:16], in_max=bs[:, h, 8:16], in_values=tmp2), reads=[B_TK], writes=[B_TK])
            posf = pos.rearrange("p a b -> p (a b)")
            P.op("dve", lambda e: e.tensor_single_scalar(out=k1u, in_=posf, scalar=4, op=ALU.logical_shift_right), reads=[B_TK], writes=[B_TK])
            P.op("dve", lambda e: e.tensor_single_scalar(out=k2u, in_=posf, scalar=15, op=ALU.bitwise_and), reads=[B_TK], writes=[B_TK])
            P.op("dve", lambda e: e.tensor_copy(out=k1f, in_=k1u), reads=[B_TK], writes=[B_TK])
            P.op("dve", lambda e: e.tensor_copy(out=k2f, in_=k2u), reads=[B_TK], writes=[B_TK])
            if stage <= 3.7:
                P.emit(finals)
                return nc, es
            for (kf, eq, pp, dst) in ((k1f, eq1, 0, 0), (k2f, eq2, 1, 1)):
                P.op("dve", lambda e, kf=kf, eq=eq: e.tensor_tensor(out=eq, in0=cap(iota16, 0, [[0, 128], [1, 16]]), in1=cap(kf, 0, [[1, 128], [0, 16]]), op=ALU.is_equal),
                     reads=[B_TK, B_C, B_AB], writes=[B_PL])
                for h in range(8):
                    P.op("pool", lambda e, eq=eq, h=h, pp=pp: e.tensor_tensor(out=eq[:, h * 16:(h + 1) * 16, :], in0=eq[:, h * 16:(h + 1) * 16, :],
                                                                        in1=cap(tixf, (2 * h + pp) * 16, [[0, 16], [1, 16]]), op=ALU.mult), reads=[B_PL], writes=[B_PL])
                P.op("dve", lambda e, eq=eq, dst=dst: e.tensor_reduce(out=abg[:, dst, :], in_=eq, axis=mybir.AxisListType.X, op=ALU.add), reads=[B_PL], writes=[B_AB])
            if stage <= 3.8:
                P.emit(finals)
                return nc, es
            P.op("dve", lambda e: e.tensor_tensor(out=ee, in0=bs, in1=cap(bs, 0, [[16, 8], [0, 16]]), op=ALU.subtract), reads=[B_TK], writes=[B_EE])
            P.op("act", lambda e: e.activation(out=ee, in_=ee, func=AF.Exp), reads=[B_EE], writes=[B_EE])
            P.op("dve", lambda e: e.tensor_reduce(out=zz[:, 0, :], in_=ee, axis=mybir.AxisListType.X, op=ALU.add), reads=[B_EE], writes=[B_EE])
            P.op("dve", lambda e: e.reciprocal(out=zz[:, 1, :], in_=zz[:, 0, :]), reads=[B_EE], writes=[B_EE])
            P.op("dve", lambda e: e.tensor_tensor(out=abg[:, 2, :].rearrange("p (h j) -> p h j", h=8), in0=ee, in1=cap(zz, 8, [[1, 8], [0, 16]]), op=ALU.mult),
                 reads=[B_EE], writes=[B_AB])
            if "abg" in dbg_d:
                dbg_out("abg", abg.rearrange("p a b -> p (a b)"), [B_AB], i * 128, 128, 0, 384)
            if stage <= 3.9:
                P.emit(finals)
                return nc, es
            tb = 6 + i % 2
            for q in range(3):
                P.op("pe", lambda e, q=q, tb=tb: e.transpose(bank(tb, q * 128, (q + 1) * 128), abg[:, q, :], identf), reads=[B_AB, B_C], writes=[PB[tb]], sig=(q == 2))
            P.op("act", lambda e, tb=tb, i=i: e.activation(out=abgT[:, :, i * 128:(i + 1) * 128], in_=bank(tb, 0, 384).rearrange("p (q t) -> p q t", q=3), func=AF.Copy),
                 reads=[PB[tb]], writes=[B_ABG])
            if stage <= 3.95 and i + 1 >= PK_TILES:
                P.emit(finals)
                return nc, es
    if stage <= 4:
        P.emit(finals)
        return nc, es

    P.barrier()
    A.top = pk_mark
    GTh = [A.alloc([64, 256], BF16) for _ in range(2)]
    B_GTH = [Buf("GTlo"), Buf("GThi")]
    RAh = [A.alloc([16, 64], BF16) for _ in range(2)]
    LBe = [A.alloc([16, 128], BF16) for _ in range(2)]
    LB = [A.alloc([16, 128], BF16) for _ in range(2)]
    B_RA = [Buf("RA0"), Buf("RA1")]
    B_LBE = [Buf("LBe0"), Buf("LBe1")]
    B_LB = [Buf("LB0"), Buf("LB1")]
    NSB = 3
    uTb = [A.alloc([4, 8, 128], BF16) for _ in range(NSB)]
    vbuf = [A.alloc([4, D], BF16) for _ in range(NSB)]
    B_UTB = [Buf("utb%d" % i) for i in range(NSB)]
    B_VBUF = [Buf("vbuf%d" % i) for i in range(NSB)]
    B_GS = [Buf("gslot%d" % i) for i in range(4)]
    gl = [A.alloc([2, 256], BF16) for _ in range(2)]
    B_GL = [Buf("gl0"), Buf("gl1")]
    hidT = [A.alloc([256], BF16) for _ in range(4)]
    B_HID = [Buf("hid%d" % i) for i in range(4)]
    x2t = [A.alloc([D], F32) for _ in range(2)]
    B_X2 = [Buf("x2t0"), Buf("x2t1")]
    yt = A.alloc([D], F32)
    B_YT = Buf("yt")
    gfb = A.alloc([D], F32)
    junk_ref[0] = A.alloc([D], BF16)
    P.op("sp", lambda e: e.dma_start(out=gfb, in_=dap(gf_d, 0, [[0, 128], [1, D]])), writes=[B_C], dma=True, semkey=("c", 50))
    B_OUT = [Buf("out%d" % i) for i in range(NT)]
    NB = 8
    _gq = [0]
    _gbk = [0]

    def build_vec(X, half, s_):
        q_ = s_ % 2
        t0 = X * 256 + s_ * 16
        P.op("dve", lambda e, t0=t0, q_=q_, half=half: e.tensor_tensor(out=RAh[q_], in0=cap(iotab, half * 64, [[0, 16], [1, 64]]),
                                                                 in1=cap(abgT, 0 * S + t0, [[1, 16], [0, 64]]), op=ALU.is_equal),
             reads=[B_ABG, B_C], writes=[B_RA[q_]])
        P.op("dve", lambda e, t0=t0, q_=q_: e.tensor_tensor(out=LBe[q_], in0=cap(iotab, 0, [[0, 16], [1, 128]]), in1=cap(abgT, 1 * S + t0, [[1, 16], [0, 128]]), op=ALU.is_equal),
             reads=[B_ABG, B_C], writes=[B_LBE[q_]])
        P.op("pool", lambda e, t0=t0, q_=q_: e.tensor_tensor(out=LB[q_], in0=LBe[q_], in1=cap(abgT, 2 * S + t0, [[1, 16], [0, 128]]), op=ALU.mult),
             reads=[B_ABG, B_LBE[q_]], writes=[B_LB[q_]])

    def build_pe(X, half, s_):
        q_ = s_ % 2
        for t8 in range(2):
            gb = t8
            for tq in range(8):
                t = t8 * 8 + tq
                P.op("pe", lambda e, t=t, gb=gb, tq=tq, q_=q_: e.matmul(bank(gb, tq * 64, (tq + 1) * 64), lhsT=LB[q_][:, t, :], rhs=RAh[q_][:, t, :], start=True, stop=True),
                     reads=[B_LB[q_], B_RA[q_]], writes=[PB[gb]], sig=(tq == 7))
            tl = s_ * 16 + t8 * 8
            evac(cap(GTh[half], tl, [[256, 64], [1, 8]]), cap(psA, gb * 512, [[1, 64], [64, 8]]), [PB[gb]], [B_GTH[half]], eng="act")

    for s_ in range(16):
        build_vec(0, 0, s_)
        build_pe(0, 0, s_)

    for blk in range(NB):
        tb0 = blk * 256

        def emit_act(pr):
            ab = 2 + pr % 2
            for hf in range(2):
                i1 = pr * 2 + hf
                grp, ec = i1 // 4, i1 % 4
                s_ = (blk * 32 + grp) % NSB
                if ec == 0:
                    P.op("sp", lambda e, grp=grp, s_=s_: e.dma_start(out=uTb[s_].rearrange("p a k e -> p a (k e)"),
                                                                  in_=dap(uts_d, grp * 4 * 131072, [[1024, 128], [131072, 4], [1, 1024]])),
                         reads=B_UTS[grp * 4:(grp + 1) * 4], writes=[B_UTB[s_]], dma=True, semkey=("utb", s_))
                    P.op("sp", lambda e, grp=grp, s_=s_: e.dma_start(out=vbuf[s_], in_=dap(vs_d, grp * 512 * 1024, [[1024, 128], [128 * 1024, 4], [1, 1024]])),
                         reads=[B_VS[grp]], writes=[B_VBUF[s_]], dma=True, semkey=("vbuf", s_))
                for k in range(8):
                    P.op("pe", lambda e, k=k, ab=ab, hf=hf, ec=ec, s_=s_, tb0=tb0: e.matmul(bank(ab, hf * 256, (hf + 1) * 256), lhsT=uTb[s_][:, ec, k, :],
                                                                                      rhs=xnT[:, k, tb0:tb0 + 256], start=(k == 0), stop=(k == 7)),
                         reads=[B_UTB[s_], B_XNT[tb0 // 512]], writes=[PB[ab]], sig=(k == 7))

        def emit_out(pr):
            ab = 2 + pr % 2
            gs = pr % 2
            P.op("act", lambda e, ab=ab, gs=gs: e.activation(out=gl[gs].rearrange("p a b -> p (a b)"), in_=bank(ab), func=AF.Gelu), reads=[PB[ab]], writes=[B_GL[gs]])
            for hf in range(2):
                i1 = pr * 2 + hf
                grp, ec = i1 // 4, i1 % 4
                s_ = (blk * 32 + grp) % NSB
                hs = i1 % 4
                gh = i1 // 64
                P.op("dve", lambda e, gs=gs, hf=hf, i1=i1, hs=hs, gh=gh: e.tensor_tensor(out=hidT[hs], in0=gl[gs][:, hf, :], in1=GTh[gh][:, i1 % 64, :], op=ALU.mult),
                     reads=[B_GL[gs], B_GTH[gh]], writes=[B_HID[hs]])
                for tt in range(2):
                    for h2 in range(2):
                        ob = 4 + tt * 2 + h2
                        P.op("pe", lambda e, ob=ob, hs=hs, tt=tt, h2=h2, ec=ec, s_=s_, i1=i1: e.matmul(
                            bank(ob), lhsT=hidT[hs][:, tt * 128:(tt + 1) * 128], rhs=vbuf[s_][:, ec, h2 * 512:(h2 + 1) * 512],
                            start=(i1 == 0), stop=(i1 == 127)), reads=[B_HID[hs], B_VBUF[s_]], writes=[PB[ob]], sig=(tt == 1 and h2 == 1))

        emit_act(0)
        for pr in range(64):
            if pr + 1 < 64:
                emit_act(pr + 1)
            hph = pr // 32
            X, half = (blk, 1) if hph == 0 else (blk + 1, 0)
            j = pr % 32
            if X < NB:
                if j % 2 == 0:
                    if j >= 2:
                        build_pe(X, half, j // 2 - 1)
                    build_vec(X, half, j // 2)
                if j == 31:
                    build_pe(X, half, 15)
            emit_out(pr)

        for tt in range(2):
            i = blk * 2 + tt
            s_ = i % 2
            P.op("sp", lambda e, i=i, s_=s_: e.dma_start(out=x2t[s_], in_=x1s_d.ap()[i * 128:(i + 1) * 128, :]), reads=[B_X1S[i]], writes=[B_X2[s_]],
                 dma=True, semkey=("x2t", s_))
            for h2 in range(2):
                ob = 4 + tt * 2 + h2
                P.op("dve", lambda e, s_=s_, h2=h2, ob=ob: e.tensor_tensor(out=x2t[s_][:, h2 * 512:(h2 + 1) * 512], in0=x2t[s_][:, h2 * 512:(h2 + 1) * 512], in1=bank(ob), op=ALU.add),
                     reads=[PB[ob]], writes=[B_X2[s_]])
            if "x2" in dbg_d:
                dbg_out("x2", x2t[s_], [B_X2[s_]], i * 128, 128, 0, D)
            rms_tile(i, x2t[s_], gfb, s_, [B_X2[s_]], B_YT, yt)
            o = P.op("sp", lambda e, i=i: e.dma_start(out=out_d.ap()[i * 128:(i + 1) * 128, :], in_=yt), reads=[B_YT], writes=[B_OUT[i]], dma=True, semkey=("yt",))
            finals.append(o)

    P.emit(finals)
    return nc, es


def _host_consts(inputs):
    c = {}
    c["identb"] = np.eye(128, dtype=np.float32).astype(ml_dtypes.bfloat16)
    c["identf"] = np.eye(128, dtype=np.float32)
    c["iotab"] = np.tile(np.arange(128, dtype=np.float32)[None, :], (128, 1)).astype(ml_dtypes.bfloat16)
    c["iota16"] = np.tile(np.arange(16, dtype=np.float32)[None, :], (128, 1))
    rpb = np.asarray(inputs["na_rpb"], np.float32)[0]
    kc = np.arange(64)[:, None]
    cq = np.arange(64)[None, :]
    dci = np.clip(kc - cq, -15, 15) + 15
    g = rpb[:, :, dci]
    lo = g[:, 0:14].transpose(2, 1, 0, 3)
    hi = g[:, 1:15].transpose(2, 1, 0, 3)
    c["rbg"] = np.ascontiguousarray(np.concatenate([lo, hi], axis=0).reshape(128, 8 * 14 * 64), np.float32)
    cs = np.clip(np.arange(64) - 8, 0, 48)
    inwin = (kc >= cs[None, :]) & (kc < cs[None, :] + 16)
    nm = np.where(inwin, 0.0, NEG).astype(np.float32)
    c["negmask"] = np.ascontiguousarray(np.concatenate([nm, nm], axis=0))
    c["gq"] = np.ascontiguousarray(np.tile(np.asarray(inputs["gqa_q_norm_g"], np.float32)[0], 2).reshape(128, 1))
    c["gk"] = np.ascontiguousarray(np.tile(np.asarray(inputs["gqa_k_norm_g"], np.float32)[0], 2).reshape(128, 1))
    t = np.arange(S)
    inv = (10000.0 ** (-np.arange(16, dtype=np.float32) / 16)).astype(np.float32)
    ang_r = (t // 64).astype(np.float32)[None, :] * inv[:, None]
    ang_c = (t % 64).astype(np.float32)[None, :] * inv[:, None]
    ang = np.concatenate([ang_r, ang_r, ang_c, ang_c], axis=0)
    ang = np.concatenate([ang, ang], axis=0).astype(np.float32)
    c["rope_c"] = np.cos(ang).astype(np.float32)
    c["rope_s"] = np.sin(ang).astype(np.float32)
    R = np.zeros((128, 128), np.float32)
    for m in range(128):
        if m % 32 < 16:
            R[m, m + 16] = -1.0
        else:
            R[m, m - 16] = 1.0
    c["permT"] = np.ascontiguousarray(R.T)
    blk = np.zeros((128, 128), np.float32)
    blk[:64, :64] = 1.0 / 64
    blk[64:, 64:] = 1.0 / 64
    c["blk64"] = blk
    return c


def _in_maps(inputs, consts):
    shared = {
        "norm1_g": np.asarray(inputs["norm1_g"], np.float32).reshape(1, D),
        "w_in": np.asarray(inputs["w_in"], np.float32).reshape(D, 4352),
        "w_proj_a": np.asarray(inputs["w_proj_a"], np.float32).reshape(512, D),
        "w_proj_b": np.asarray(inputs["w_proj_b"], np.float32).reshape(512, D),
        "w_out": np.asarray(inputs["w_out"], np.float32).reshape(D, D),
        "norm2_g": np.asarray(inputs["norm2_g"], np.float32).reshape(1, D),
        "peer_w_q": np.asarray(inputs["peer_w_q"], np.float32).reshape(D, 2048),
        "peer_sub_keys": np.asarray(inputs["peer_sub_keys"], np.float32).reshape(16 * 128, 128),
        "peer_u": np.asarray(inputs["peer_u"], np.float32).reshape(16384, D),
        "peer_v": np.asarray(inputs["peer_v"], np.float32).reshape(16384, D),
        "norm_f_g": np.asarray(inputs["norm_f_g"], np.float32).reshape(1, D),
    }
    shared.update(consts)
    x = np.asarray(inputs["x"], np.float32)
    return [dict(shared, x=np.ascontiguousarray(x[b])) for b in range(8)]


def kernel(**inputs):
    nc, es = build()
    with es:
        pass
    maps = _in_maps(inputs, _host_consts(inputs))
    res = run_bass_kernel_spmd(nc, maps, core_ids=list(range(8)))
    return np.stack([r["out"] for r in res.results], axis=0).astype(np.float32)
```

```python
import contextlib
import numpy as np
import ml_dtypes
import concourse.bass as bass
import concourse.mybir as mybir
from concourse.bass_utils import run_bass_kernel_spmd

F32 = mybir.dt.float32
BF16 = mybir.dt.bfloat16
U32 = mybir.dt.uint32
ALU = mybir.AluOpType
AF = mybir.ActivationFunctionType

S = 2048
D = 1024
NT = 16
EPS = 1e-6
NEG = -30000.0


class Buf:
    __slots__ = ("name", "last_w", "readers", "writers")

    def __init__(self, name):
        self.name = name
        self.last_w = None
        self.readers = []
        self.writers = []


class Prog:
    ENGS = ("pe", "dve", "act", "pool", "sp")

    def __init__(self, nc, es):
        self.nc = nc
        self.es = es
        self.ops = []
        self.sem = {}
        self.cnt = {}
        self.opsig = []
        self.last_eng = {}
        self.last_dma = {}
        self.pending = {}
        self.sigflag = []

    def _sem(self, key):
        if key not in self.sem:
            self.sem[key] = self.es.enter_context(self.nc.semaphore("s%d" % len(self.sem)))
            self.cnt[key] = 0
        return self.sem[key]

    def _compact(self, lst):
        if len(lst) <= 48:
            return lst
        best = {}
        keep = []
        for o in lst:
            sg = self.opsig[o]
            if sg is None:
                keep.append(o)
            elif sg[0] not in best or self.opsig[best[sg[0]]][1] < sg[1]:
                best[sg[0]] = o
        return keep + list(best.values())

    def op(self, eng, fn, reads=(), writes=(), dma=False, semkey=None, extra_deps=(), sig=True, wdis=()):
        deps = set(extra_deps)
        for b in reads:
            if b.last_w is not None:
                deps.add(b.last_w)
            deps.update(b.writers)
        for b in writes:
            if b.last_w is not None:
                deps.add(b.last_w)
            deps.update(b.writers)
            deps.update(b.readers)
        for b in wdis:
            if b.last_w is not None:
                deps.add(b.last_w)
            deps.update(b.readers)
        oid = len(self.ops)
        if dma:
            key = ("dma", semkey)
            inc = 16
            self.last_dma[key] = oid
        else:
            key = ("eng", eng)
            inc = 1
            self.last_eng[eng] = oid
        self._sem(key)
        if sig:
            self.cnt[key] += inc
            self.opsig.append((key, self.cnt[key]))
            for po in self.pending.pop(key, []):
                self.opsig[po] = (key, self.cnt[key])
        else:
            assert not dma
            self.opsig.append(None)
            self.pending.setdefault(key, []).append(oid)
        self.sigflag.append(sig)
        self.ops.append((eng, fn, sorted(deps), dma))
        for b in reads:
            b.readers.append(oid)
            b.readers = self._compact(b.readers)
        for b in writes:
            b.last_w = oid
            b.readers = []
            b.writers = []
        for b in wdis:
            b.writers.append(oid)
            b.writers = self._compact(b.writers)
        return oid

    def barrier(self):
        deps = list(self.last_eng.values()) + list(self.last_dma.values())
        for eng in self.ENGS:
            self.op(eng, None, extra_deps=deps)

    def emit(self, final_wait_ops=()):
        nc = self.nc
        assert not any(self.pending.values()), "unsignalled trailing ops"
        block = self.es.enter_context(nc.Block())
        per_eng = {e: [] for e in self.ENGS}
        for oid, (eng, fn, deps, dma) in enumerate(self.ops):
            per_eng[eng].append(oid)
        prog = self

        def make(engname):
            def body(e):
                waited = {}
                for oid in per_eng[engname]:
                    eng, fn, deps, dma = prog.ops[oid]
                    need = {}
                    for d in deps:
                        deng, _, _, ddma = prog.ops[d]
                        if (not ddma) and deng == engname and engname == "pe":
                            continue
                        k, v = prog.opsig[d]
                        if need.get(k, 0) < v:
                            need[k] = v
                    for k, v in need.items():
                        if waited.get(k, 0) < v:
                            e.wait_ge(prog.sem[k], v)
                            waited[k] = v
                    k, v = prog.opsig[oid]
                    if fn is None:
                        e.sem_inc(prog.sem[k], 1)
                    else:
                        ins = fn(e)
                        if prog.sigflag[oid]:
                            ins.then_inc(prog.sem[k], 16 if dma else 1)
                if engname == "sp":
                    need = {}
                    for d in final_wait_ops:
                        k, v = prog.opsig[d]
                        if need.get(k, 0) < v:
                            need[k] = v
                    for k, v in need.items():
                        e.wait_ge(prog.sem[k], v)
            return body

        block.tensor(make("pe"))
        block.vector(make("dve"))
        block.scalar(make("act"))
        block.gpsimd(make("pool"))
        block.sync(make("sp"))


def _dtsize(dt):
    return 2 if dt == BF16 else 4


class Arena:
    def __init__(self, ar, nbytes):
        self.ar = ar
        self.nbytes = nbytes
        self.top = 0

    def alloc(self, shape, dt):
        n = int(np.prod(shape))
        nb = n * _dtsize(dt)
        off = self.top
        self.top += (nb + 63) // 64 * 64
        assert self.top <= self.nbytes, ("arena overflow", self.top, self.nbytes)
        v = self.ar[:, off // 2:(off + nb) // 2]
        if dt != BF16:
            v = v.bitcast(dt)
        if len(shape) == 2:
            v = v.rearrange("p (a b) -> p a b", a=shape[0])
        elif len(shape) == 3:
            v = v.rearrange("p (a b c) -> p a b c", a=shape[0], b=shape[1])
        elif len(shape) == 4:
            v = v.rearrange("p (a b c d) -> p a b c d", a=shape[0], b=shape[1], c=shape[2])
        return v


def cap(view, rel, dims, parts=None, pstart=0):
    ps = view.ap[0][0]
    npart = parts if parts is not None else view.ap[0][1]
    return bass.AP(tensor=view.tensor, offset=view.offset + pstart * ps + rel,
                   ap=[[ps, npart]] + [list(d) for d in dims])


def dap(t, off, dims):
    return bass.AP(tensor=t, offset=off, ap=[list(d) for d in dims])


ARENA_BYTES = 207 * 1024


NA_SUB = 9
NA_ROWS = 32
PK_TILES = 1


def build(stage=99, dbg=()):
    nc = bass.Bass("TRN2", target_bir_lowering=False)
    es = contextlib.ExitStack()

    def din(name, shape, dt=F32):
        return nc.dram_tensor(name, list(shape), dt, kind="ExternalInput")

    x_d = din("x", [S, D])
    g1_d = din("norm1_g", [1, D])
    win_d = din("w_in", [D, 4352])
    rb_d = din("rbg", [128, 8 * 14 * 64])
    nm_d = din("negmask", [128, 64])
    gq_d = din("gq", [128, 1])
    gk_d = din("gk", [128, 1])
    wpa_d = din("w_proj_a", [512, D])
    wpb_d = din("w_proj_b", [512, D])
    wo_d = din("w_out", [D, D])
    g2_d = din("norm2_g", [1, D])
    wq_d = din("peer_w_q", [D, 2048])
    sk_d = din("peer_sub_keys", [16 * 128, 128])
    u_d = din("peer_u", [16384, D])
    v_d = din("peer_v", [16384, D])
    gf_d = din("norm_f_g", [1, D])
    idb_d = din("identb", [128, 128], BF16)
    idf_d = din("identf", [128, 128])
    iob_d = din("iotab", [128, 128], BF16)
    io16_d = din("iota16", [128, 16])
    ropec_d = din("rope_c", [128, S])
    ropes_d = din("rope_s", [128, S])
    perm_d = din("permT", [128, 128])
    blk_d = din("blk64", [128, 128])
    out_d = nc.dram_tensor("out", [S, D], F32, kind="ExternalOutput")
    x1s_d = nc.dram_tensor("x1s", [S, D], F32, kind="Internal")
    vs_d = nc.dram_tensor("vs", [16384, D], BF16, kind="Internal")
    uts_d = nc.dram_tensor("uts", [128, 128 * 1024], BF16, kind="Internal")
    dbg_d = {}
    for name, shape in dbg:
        dbg_d[name] = nc.dram_tensor("dbg_" + name, list(shape), F32, kind="ExternalOutput")

    ar = es.enter_context(nc.sbuf_tensor("arena", [128, ARENA_BYTES // 2], BF16))
    ps = es.enter_context(nc.psum_tensor("ps", [128, 4096], F32))
    A = Arena(ar, ARENA_BYTES)
    psA = ps[:, :]
    P = Prog(nc, es)
    PB = [Buf("bank%d" % i) for i in range(8)]

    def bank(i, a=0, b=512):
        return ps[:, i * 512 + a:i * 512 + b]

    def bankb(i):
        return ps[:, i * 512:(i + 1) * 512].bitcast(BF16)

    _dq = [0]

    def dma_q():
        _dq[0] += 1
        return "sp"

    identb = A.alloc([128], BF16)
    identf = A.alloc([128], F32)
    iotab = A.alloc([128], BF16)
    iota16 = A.alloc([16], F32)
    B_C = Buf("consts")

    def ld(dst, src, q="sp", key=None):
        P.op(q, lambda e: e.dma_start(out=dst, in_=src), writes=[B_C], dma=True, semkey=("c", key or id(dst)))

    ld(identb, idb_d.ap(), key=0)
    ld(identf, idf_d.ap(), key=1)
    ld(iotab, iob_d.ap(), key=2)
    ld(iota16, io16_d.ap(), key=3)
    stats = A.alloc([64], F32)
    B_ST = [Buf("st%d" % i) for i in range(NT)]
    base_mark = A.top

    hT = A.alloc([8, S], BF16)
    B_HT = [Buf("hT%d" % i) for i in range(4)]
    wbf_mark = A.top
    wbf = [A.alloc([8, 512], BF16) for _ in range(2)]
    B_WB = [Buf("wb0"), Buf("wb1")]
    att_mark = A.top

    def dump(name, view_fn, reads, rows, cols):
        pass

    g1b = A.alloc([D], F32)
    ld(g1b, dap(g1_d, 0, [[0, 128], [1, D]]), key=4)
    xt = [A.alloc([D], F32) for _ in range(2)]
    hb = [A.alloc([D], BF16) for _ in range(2)]
    junk_ref = [A.alloc([D], BF16)]
    B_XT = [Buf("xt0"), Buf("xt1")]
    B_HB = [Buf("hb0"), Buf("hb1")]
    B_JK = Buf("junk")
    ss, rt, rstd = stats[:, 0:16], stats[:, 16:32], stats[:, 32:48]

    def rms_tile(i, src_view, gvec, s, reads_src, B_hb_s, hb_s):
        jk = junk_ref[0]
        P.op("act", lambda e: e.activation(out=jk, in_=src_view, func=AF.Square, accum_out=ss[:, i:i + 1]),
             reads=reads_src, writes=[B_JK, B_ST[i]])
        P.op("act", lambda e: e.activation(out=rt[:, i:i + 1], in_=ss[:, i:i + 1], func=AF.Sqrt, scale=1.0 / D, bias=EPS),
             reads=[B_ST[i]], writes=[B_ST[i]])
        P.op("dve", lambda e: e.reciprocal(out=rstd[:, i:i + 1], in_=rt[:, i:i + 1]), reads=[B_ST[i]], writes=[B_ST[i]])
        P.op("dve", lambda e: e.scalar_tensor_tensor(out=hb_s, in0=src_view, scalar=rstd[:, i:i + 1], in1=gvec,
                                                     op0=ALU.mult, op1=ALU.mult),
             reads=reads_src + [B_ST[i], B_C], writes=[B_hb_s])

    def transpose_tile_to(i, hb_s, B_hb_s, dstT, B_dst, bk):
        pb = bankb(bk)
        for k in range(8):
            P.op("pe", lambda e, k=k: e.transpose(pb[:, k * 128:(k + 1) * 128], hb_s[:, k * 128:(k + 1) * 128], identb),
                 reads=[B_hb_s, B_C], writes=[PB[bk]], sig=(k == 7))
        P.op("act", lambda e: e.activation(out=dstT[:, :, i * 128:(i + 1) * 128],
                                           in_=pb.rearrange("p (k t) -> p k t", k=8), func=AF.Copy),
             reads=[PB[bk]], writes=[B_dst])

    for i in range(NT):
        s = i % 2
        P.op("sp", lambda e, i=i, s=s: e.dma_start(out=xt[s], in_=x_d.ap()[i * 128:(i + 1) * 128, :]),
             writes=[B_XT[s]], dma=True, semkey=("xt", s))
        rms_tile(i, xt[s], g1b, s, [B_XT[s]], B_HB[s], hb[s])
        transpose_tile_to(i, hb[s], B_HB[s], hT, B_HT[i // 4], 6 + s)

    finals = []
    B_DBG = Buf("dbg")

    def dbg_out(name, sb_view, reads, r0, nrows, c0, ncols):
        if name in dbg_d:
            o = P.op("sp", lambda e: e.dma_start(out=dbg_d[name].ap()[r0:r0 + nrows, c0:c0 + ncols], in_=sb_view),
                     reads=reads, writes=[B_DBG], dma=True, semkey=("dbg",))
            finals.append(o)

    if "hT" in dbg_d:
        tmpf = A.alloc([S], F32)
        B_T = Buf("tmpf")
        for k in range(8):
            P.op("dve", lambda e, k=k: e.tensor_copy(out=tmpf, in_=hT[:, k, :]), reads=B_HT, writes=[B_T])
            dbg_out("hT", tmpf, [B_T], k * 128, 128, 0, S)

    if stage <= 0:
        P.emit(finals)
        return nc, es

    P.barrier()
    A.top = att_mark
    yaT = A.alloc([4, S], BF16)
    B_YAT, B_YBT = Buf("yaT"), Buf("ybT")
    if dbg_d:
        P.op("pool", lambda e: e.memset(yaT.rearrange("p a b -> p (a b)"), 0.0), writes=[B_YAT])
    ph_mark = A.top
    QaT = A.alloc([4, S], BF16)
    KaT = A.alloc([8, S], BF16)
    Va = A.alloc([16, 8, 65], BF16)
    Vas = A.alloc([15, 8, 65], BF16)
    RB2 = A.alloc([14, 8, 64], F32)
    negm = A.alloc([64], F32)
    B_QA, B_KA, B_VA, B_VAS, B_RB = Buf("QaT"), Buf("KaT"), Buf("Va"), Buf("Vas"), Buf("RB2")

    P.op("sp", lambda e: e.dma_start(out=RB2.rearrange("p a b c -> p (a b c)"), in_=rb_d.ap()), writes=[B_RB], dma=True, semkey=("c", 10))
    P.op("sp", lambda e: e.dma_start(out=negm, in_=nm_d.ap()), writes=[B_RB], dma=True, semkey=("c", 11))
    P.op("pool", lambda e: e.tensor_tensor(out=RB2.rearrange("p a b c -> p (a b) c"), in0=RB2.rearrange("p a b c -> p (a b) c"),
                                           in1=cap(negm, 0, [[0, 112], [1, 64]]), op=ALU.add), reads=[B_RB], writes=[B_RB])
    P.op("pool", lambda e: e.memset(KaT.rearrange("p a b -> p (a b)"), 0.0), writes=[B_KA])
    P.op("pool", lambda e: e.memset(Va.rearrange("p a b c -> p (a b c)"), 1.0), writes=[B_VA])
    P.op("pool", lambda e: e.memset(Vas.rearrange("p a b c -> p (a b c)"), 1.0), writes=[B_VAS])

    def load_w(slot, src_t, row_stride, c0, ncols, kch=8, dcol=0):
        dst = wbf[slot][:, 0:kch, dcol:dcol + ncols]
        P.op("pool", lambda e: e.dma_start(out=dst, in_=dap(src_t, c0, [[row_stride, 128], [128 * row_stride, kch], [1, ncols]])),
             writes=[B_WB[slot]], dma=True, semkey=("wb", slot))

    _bk = [0]

    def next_bank(n=6):
        b = _bk[0] % n
        _bk[0] += 1
        return b

    _ev = [0]

    def evac(out_view, in_view, reads, writes, eng=None):
        _ev[0] += 1
        if eng == "act" or (eng is None and _ev[0] % 2 == 0):
            P.op("act", lambda e: e.activation(out=out_view, in_=in_view, func=AF.Copy), reads=reads, wdis=writes)
        else:
            P.op("dve", lambda e: e.tensor_copy(out=out_view, in_=in_view), reads=reads, wdis=writes)

    def proj_fm(slot, ncols, dstT, B_dst, post=None):
        for c in range(ncols // 128):
            for tc in range(4):
                b = next_bank()
                for k in range(8):
                    P.op("pe", lambda e, k=k, c=c, tc=tc, b=b: e.matmul(bank(b), lhsT=wbf[slot][:, k, c * 128:(c + 1) * 128],
                                                                        rhs=hT[:, k, tc * 512:(tc + 1) * 512], start=(k == 0), stop=(k == 7)),
                         reads=[B_WB[slot], B_HT[tc]], writes=[PB[b]], sig=(k == 7))
                if post is None:
                    evac(dstT[:, c, tc * 512:(tc + 1) * 512], bank(b), [PB[b]], [B_dst])
                else:
                    post(c, tc, b)

    def proj_tm(slot, ncols, tok_off, ntiles, dst_fn, B_dst):
        for i in range(ntiles):
            b = next_bank()
            t0 = tok_off + i * 128
            for k in range(8):
                P.op("pe", lambda e, k=k, b=b, t0=t0: e.matmul(bank(b, 0, ncols), lhsT=hT[:, k, t0:t0 + 128],
                                                               rhs=wbf[slot][:, k, 0:ncols], start=(k == 0), stop=(k == 7)),
                     reads=[B_WB[slot]] + B_HT, writes=[PB[b]], sig=(k == 7))
            evac(dst_fn(i), bank(b, 0, ncols).rearrange("p (h d) -> p h d", d=64), [PB[b]], [B_dst])

    load_w(0, win_d, 4352, 0, 512)
    load_w(1, win_d, 4352, 512, 512)
    proj_fm(0, 512, QaT, B_QA)
    load_w(0, win_d, 4352, 1024, 512)
    def ka_post(c, tc, b):
        evac(KaT[0:64, 2 * c, tc * 512:(tc + 1) * 512], ps[0:64, b * 512:(b + 1) * 512], [PB[b]], [B_KA])
        evac(KaT[64:128, 2 * c + 1, tc * 512:(tc + 1) * 512], ps[64:128, b * 512:(b + 1) * 512], [PB[b]], [B_KA])

    proj_fm(1, 512, KaT, B_KA, post=ka_post)
    proj_tm(0, 512, 0, 16, lambda i: Va[:, i, :, 0:64], B_VA)
    proj_tm(0, 512, 64, 15, lambda i: Vas[:, i, :, 0:64], B_VAS)
    load_w(1, win_d, 4352, 1536, 512)

    if stage <= 0.5:
        tmpf = A.alloc([S], F32)
        B_T = Buf("tmpf2")
        if "QaT" in dbg_d:
            for k in range(4):
                P.op("dve", lambda e, k=k: e.tensor_copy(out=tmpf, in_=QaT[:, k, :]), reads=[B_QA], writes=[B_T])
                dbg_out("QaT", tmpf, [B_T], k * 128, 128, 0, S)
        if "Va" in dbg_d:
            for i in range(16):
                P.op("dve", lambda e, i=i: e.tensor_copy(out=tmpf[:, 0:512].rearrange("p (h d) -> p h d", d=64), in_=Va[:, i, :, 0:64]), reads=[B_VA], writes=[B_T])
                dbg_out("Va", tmpf[:, 0:512], [B_T], i * 128, 128, 0, 512)
        P.emit(finals)
        return nc, es
    ssb = [A.alloc([1024], F32) for _ in range(2)]
    PT = [A.alloc([1024], BF16) for _ in range(2)]
    yar = [A.alloc([512], BF16) for _ in range(2)]
    rec = A.alloc([2, 4], F32)
    B_SSB = [Buf("ssb0"), Buf("ssb1")]
    B_PT = [Buf("pt0"), Buf("pt1")]
    B_YAR = [Buf("yar0"), Buf("yar1")]
    B_REC = [Buf("rec0"), Buf("rec1")]
    na_steps = [(r, g) for r in range(32) for g in range(2)]

    def na_A(step):
        r, g = na_steps[step]
        rs = min(max(r - 4, 0), 24)
        ks = rs * 64
        dr0 = rs - r + 7
        sl = step % 2
        sb0 = sl * 2
        for hh in range(4):
            h = g * 4 + hh
            for kt in range(4):
                o0 = sb0 * 512 + kt * 256 + hh * 64
                bkw = sb0 + (kt // 2)
                P.op("pe", lambda e, o0=o0, h=h, kt=kt, ks=ks, r=r: e.matmul(
                    ps[:, o0:o0 + 64], lhsT=KaT[:, h, ks + kt * 128:ks + (kt + 1) * 128],
                    rhs=QaT[:, h // 2, r * 64:(r + 1) * 64], start=True, stop=True),
                    reads=[B_KA, B_QA], writes=[PB[bkw]], sig=(hh == 3 and kt == 3))
        P.op("dve", lambda e, sl=sl, sb0=sb0, g=g, dr0=dr0: e.scalar_tensor_tensor(
            out=ssb[sl].rearrange("p (k q) -> p k q", k=4),
            in0=ps[:, sb0 * 512:sb0 * 512 + 1024].rearrange("p (k q) -> p k q", k=4),
            scalar=0.125, in1=cap(RB2, (dr0 * 8 + g * 4) * 64, [[1024, 4], [1, 256]]),
            op0=ALU.mult, op1=ALU.add),
            reads=[PB[sb0], PB[sb0 + 1], B_RB], writes=[B_SSB[sl]])
        P.op("act", lambda e, sl=sl: e.activation(out=PT[sl], in_=ssb[sl], func=AF.Exp), reads=[B_SSB[sl]], writes=[B_PT[sl]])

    def na_B(step):
        r, g = na_steps[step]
        rs = min(max(r - 4, 0), 24)
        sl = step % 2
        ys = r % 2
        pvb = 4 + sl
        for hh in range(4):
            h = g * 4 + hh
            for kt in range(4):
                if rs % 2 == 0:
                    vsrc, bv, ti = Va, B_VA, rs // 2 + kt
                else:
                    vsrc, bv, ti = Vas, B_VAS, (rs - 1) // 2 + kt
                P.op("pe", lambda e, pvb=pvb, hh=hh, h=h, kt=kt, sl=sl, vsrc=vsrc, ti=ti: e.matmul(
                    ps[0:64, pvb * 512 + hh * 65:pvb * 512 + (hh + 1) * 65],
                    lhsT=PT[sl][:, kt * 256 + hh * 64:kt * 256 + (hh + 1) * 64], rhs=vsrc[:, ti, h, :],
                    start=(kt == 0), stop=(kt == 3)),
                    reads=[B_PT[sl], bv], writes=[PB[pvb]], sig=(hh == 3 and kt == 3))
        P.op("dve", lambda e, pvb=pvb, sl=sl: e.reciprocal(out=rec[0:64, sl, :], in_=cap(psA, pvb * 512 + 64, [[65, 4]], parts=64)),
             reads=[PB[pvb]], writes=[B_REC[sl]])
        P.op("dve", lambda e, pvb=pvb, sl=sl, g=g, ys=ys: e.tensor_tensor(
            out=yar[ys][0:64, g * 256:(g + 1) * 256].rearrange("p (h d) -> p h d", h=4),
            in0=cap(psA, pvb * 512, [[65, 4], [1, 64]], parts=64),
            in1=cap(rec, sl * 4, [[1, 4], [0, 64]], parts=64), op=ALU.mult),
            reads=[PB[pvb], B_REC[sl]], wdis=[B_YAR[ys]])
        if g == 1:
            tb = 6 + ys
            pbt = bankb(tb)
            for c in range(4):
                P.op("pe", lambda e, c=c, ys=ys, pbt=pbt: e.transpose(pbt[:, c * 64:(c + 1) * 64], yar[ys][0:64, c * 128:(c + 1) * 128], identb[0:64, 0:64]),
                     reads=[B_YAR[ys], B_C], writes=[PB[tb]], sig=(c == 3))
            P.op("act", lambda e, r=r, pbt=pbt: e.activation(out=yaT[:, :, r * 64:(r + 1) * 64], in_=pbt[:, 0:256].rearrange("p (c t) -> p c t", c=4), func=AF.Copy),
                 reads=[PB[tb]], writes=[B_YAT])

    na_A(0)
    for step in range(64):
        if step + 1 < 64:
            na_A(step + 1)
        na_B(step)

    def dump_fm(name, srcT, B_src, nch):
        if name in dbg_d:
            tmpf = A.alloc([1024], F32)
            B_T = Buf("tmpf_" + name)
            for k in range(nch):
                for hf in range(2):
                    P.op("dve", lambda e, k=k, hf=hf: e.tensor_copy(out=tmpf, in_=srcT[:, k, hf * 1024:(hf + 1) * 1024]), reads=[B_src], writes=[B_T])
                    dbg_out(name, tmpf, [B_T], k * 128, 128, hf * 1024, 1024)

    dump_fm("yaT", yaT, B_YAT, 4)
    if stage <= 1:
        P.emit(finals)
        return nc, es

    P.barrier()
    A.top = ph_mark
    ybT = A.alloc([4, S], BF16)
    if dbg_d:
        P.op("pool", lambda e: e.memset(ybT.rearrange("p a b -> p (a b)"), 0.0), writes=[B_YBT])
    mg_mark = A.top
    QbT = A.alloc([4, S], BF16)
    KbT = A.alloc([4, S], BF16)
    Vb = A.alloc([16, 2, 65], BF16)
    ropeC = A.alloc([S], F32)
    ropeS = A.alloc([S], F32)
    permT = A.alloc([128], F32)
    blk64 = A.alloc([128], F32)
    gqv = A.alloc([1], F32)
    gkv = A.alloc([1], F32)
    B_QB, B_KB, B_VB, B_RC = Buf("QbT"), Buf("KbT"), Buf("Vb"), Buf("ropec")
    for dst, src, key in ((ropeC, ropec_d, 20), (ropeS, ropes_d, 21), (permT, perm_d, 22), (blk64, blk_d, 23), (gqv, gq_d, 24), (gkv, gk_d, 25)):
        P.op("sp", lambda e, dst=dst, src=src: e.dma_start(out=dst, in_=src.ap()), writes=[B_RC], dma=True, semkey=("c", key))
    P.op("pool", lambda e: e.memset(Vb.rearrange("p a b c -> p (a b c)"), 1.0), writes=[B_VB])
    P.op("pool", lambda e: e.memset(KbT.rearrange("p a b -> p (a b)"), 0.0), writes=[B_KB])
    sqf = A.alloc([512], F32)
    rtf = A.alloc([512], F32)
    rsf = A.alloc([512], F32)
    qn = A.alloc([512], F32)
    t1 = A.alloc([512], F32)
    t2 = A.alloc([512], F32)
    B_SQ, B_RT, B_RS, B_QN, B_T1, B_T2 = (Buf(n) for n in ("sqf", "rtf", "rsf", "qn", "t1", "t2"))

    def qk_post(dstT, B_dst, gvec, kmode=False):
        def post(c, tc, b):
            b2 = next_bank()
            P.op("act", lambda e: e.activation(out=sqf, in_=bank(b), func=AF.Square), reads=[PB[b]], writes=[B_SQ])
            P.op("pe", lambda e: e.matmul(bank(b2), lhsT=blk64, rhs=sqf, start=True, stop=True), reads=[B_SQ, B_RC], writes=[PB[b2]])
            P.op("act", lambda e: e.activation(out=rtf, in_=bank(b2), func=AF.Sqrt, bias=EPS), reads=[PB[b2]], writes=[B_RT])
            P.op("dve", lambda e: e.reciprocal(out=rsf, in_=rtf), reads=[B_RT], writes=[B_RS])
            P.op("dve", lambda e: e.scalar_tensor_tensor(out=qn, in0=bank(b), scalar=gvec[:, 0:1], in1=rsf, op0=ALU.mult, op1=ALU.mult),
                 reads=[PB[b], B_RS, B_RC], writes=[B_QN])
            b3 = next_bank()
            P.op("pe", lambda e: e.matmul(bank(b3), lhsT=permT, rhs=qn, start=True, stop=True), reads=[B_QN, B_RC], writes=[PB[b3]])
            P.op("dve", lambda e: e.tensor_tensor(out=t1, in0=qn, in1=ropeC[:, tc * 512:(tc + 1) * 512], op=ALU.mult), reads=[B_QN, B_RC], writes=[B_T1])
            P.op("dve", lambda e: e.tensor_tensor(out=t2, in0=bank(b3), in1=ropeS[:, tc * 512:(tc + 1) * 512], op=ALU.mult), reads=[PB[b3], B_RC], writes=[B_T2])
            if not kmode:
                P.op("pool", lambda e: e.tensor_tensor(out=dstT[:, c, tc * 512:(tc + 1) * 512], in0=t1, in1=t2, op=ALU.add), reads=[B_T1, B_T2], writes=[B_dst])
            else:
                lo_idx = 0 if c == 0 else 2
                hi_idx = 3 if c == 0 else 1
                P.op("pool", lambda e: e.tensor_tensor(out=dstT[0:64, lo_idx, tc * 512:(tc + 1) * 512], in0=t1[0:64, :], in1=t2[0:64, :], op=ALU.add), reads=[B_T1, B_T2], writes=[B_dst])
                P.op("pool", lambda e: e.tensor_tensor(out=dstT[64:128, hi_idx, tc * 512:(tc + 1) * 512], in0=t1[64:128, :], in1=t2[64:128, :], op=ALU.add), reads=[B_T1, B_T2], writes=[B_dst])
        return post

    load_w(0, win_d, 4352, 2048, 128, dcol=0)
    load_w(0, win_d, 4352, 2112, 64, dcol=128)
    load_w(0, win_d, 4352, 2048, 64, dcol=192)
    load_w(0, win_d, 4352, 2176, 128, dcol=256)
    proj_fm(1, 512, QbT, B_QB, post=qk_post(QbT, B_QB, gqv))
    proj_fm(0, 256, KbT, B_KB, post=qk_post(KbT, B_KB, gkv, kmode=True))
    for i in range(16):
        b = next_bank()
        for k in range(8):
            P.op("pe", lambda e, k=k, b=b, i=i: e.matmul(bank(b, 0, 128), lhsT=hT[:, k, i * 128:(i + 1) * 128], rhs=wbf[0][:, k, 256:384],
                                                      start=(k == 0), stop=(k == 7)), reads=[B_WB[0]] + B_HT, writes=[PB[b]], sig=(k == 7))
        evac(Vb[:, i, :, 0:64], bank(b, 0, 128).rearrange("p (h d) -> p h d", d=64), [PB[b]], [B_VB])

    dump_fm("QbT", QbT, B_QB, 4)
    dump_fm("KbT", KbT, B_KB, 4)

    PTf = [A.alloc([16, 512], BF16) for _ in range(2)]
    B_PTF = [Buf("ptf0"), Buf("ptf1")]
    ybt = A.alloc([4, 512], BF16)
    B_YBTOK = Buf("ybtok")
    recg = A.alloc([2, 4], F32)
    B_RECG = [Buf("recg0"), Buf("recg1")]
    _sg = [0]

    def emit_S(hc, kb2):
        qc, h = hc // 8, hc % 8
        sl = _sg[0] % 2
        _sg[0] += 1
        hs = hc % 2
        kvh = h // 4
        for j in range(2):
            kt = kb2 * 2 + j
            bk = sl * 2 + j
            P.op("pe", lambda e, bk=bk, kvh=kvh, kt=kt, h=h, qc=qc: e.matmul(
                bank(bk), lhsT=KbT[:, kvh * 2 + (h % 2), kt * 128:(kt + 1) * 128],
                rhs=QbT[:, h // 2, qc * 512:(qc + 1) * 512], start=True, stop=True),
                reads=[B_KB, B_QB], writes=[PB[bk]])
        P.op("act", lambda e, sl=sl, hs=hs, kb2=kb2: e.activation(out=PTf[hs][:, kb2 * 2:kb2 * 2 + 2, :].rearrange("p a b -> p (a b)"),
                                                            in_=ps[:, sl * 1024:(sl + 1) * 1024], func=AF.Exp, scale=0.125),
             reads=[PB[sl * 2], PB[sl * 2 + 1]], wdis=[B_PTF[hs]])

    def emit_PVall(hc):
        qc, h = hc // 8, hc % 8
        hs = hc % 2
        kvh = h // 4
        pvb = 4 + hs
        for qt in range(4):
            for kt in range(16):
                P.op("pe", lambda e, pvb=pvb, qt=qt, hs=hs, kt=kt, kvh=kvh: e.matmul(
                    ps[:, pvb * 512 + qt * 65:pvb * 512 + (qt + 1) * 65], lhsT=PTf[hs][:, kt, qt * 128:(qt + 1) * 128],
                    rhs=Vb[:, kt, kvh, :], start=(kt == 0), stop=(kt == 15)),
                    reads=[B_PTF[hs], B_VB], writes=[PB[pvb]], sig=(qt == 3 and kt == 15))
        P.op("dve", lambda e, pvb=pvb, hs=hs: e.reciprocal(out=recg[:, hs, :], in_=cap(psA, pvb * 512 + 64, [[65, 4]])),
             reads=[PB[pvb]], writes=[B_RECG[hs]])
        P.op("dve", lambda e, pvb=pvb, hs=hs, h=h: e.tensor_tensor(
            out=ybt[:, :, h * 64:(h + 1) * 64], in0=cap(psA, pvb * 512, [[65, 4], [1, 64]]),
            in1=cap(recg, hs * 4, [[1, 4], [0, 64]]), op=ALU.mult),
            reads=[PB[pvb], B_RECG[hs]], writes=[B_YBTOK])
        if h == 7:
            for qt in range(4):
                tb = 6 + qt % 2
                pbt = bankb(tb)
                for c in range(4):
                    P.op("pe", lambda e, c=c, qt=qt, pbt=pbt: e.transpose(pbt[:, c * 128:(c + 1) * 128], ybt[:, qt, c * 128:(c + 1) * 128], identb),
                         reads=[B_YBTOK, B_C], writes=[PB[tb]], sig=(c == 3))
                t0 = qc * 512 + qt * 128
                P.op("act", lambda e, t0=t0, pbt=pbt: e.activation(out=ybT[:, :, t0:t0 + 128], in_=pbt[:, 0:512].rearrange("p (c t) -> p c t", c=4), func=AF.Copy),
                     reads=[PB[tb]], writes=[B_YBT])

    for hc in range(33):
        if hc < 32:
            for kb2 in range(8):
                emit_S(hc, kb2)
                if kb2 == 1 and hc > 0:
                    emit_PVall(hc - 1)
        else:
            emit_PVall(31)

    dump_fm("ybT", ybT, B_YBT, 4)
    if stage <= 2:
        P.emit(finals)
        return nc, es


    P.barrier()
    A.top = mg_mark
    mT = A.alloc([8, S], BF16)
    B_MT = Buf("mT")
    md_mark = A.top
    wpa = A.alloc([4, D], BF16)
    wpb = A.alloc([4, D], BF16)
    B_WP = Buf("wp")
    P.op("pool", lambda e: e.dma_start(out=wpa, in_=dap(wpa_d, 0, [[D, 128], [128 * D, 4], [1, D]])), writes=[B_WP], dma=True, semkey=("c", 30))
    P.op("pool", lambda e: e.dma_start(out=wpb, in_=dap(wpb_d, 0, [[D, 128], [128 * D, 4], [1, D]])), writes=[B_WP], dma=True, semkey=("c", 31))
    sga = A.alloc([512], BF16)
    sgb = A.alloc([512], BF16)
    m1 = A.alloc([512], F32)
    m2 = A.alloc([512], F32)
    B_SGA, B_SGB, B_M1, B_M2 = Buf("sga"), Buf("sgb"), Buf("m1"), Buf("m2")
    for grp in range(2):
        load_w(0, win_d, 4352, 2304 + grp * 512, 512)
        load_w(1, win_d, 4352, 3328 + grp * 512, 512)
        for cn in range(4):
            n = grp * 4 + cn
            for tc in range(4):
                for (slot, wp, yT, B_y, sg, B_sg, mm, B_mm) in ((0, wpa, yaT, B_YAT, sga, B_SGA, m1, B_M1), (1, wpb, ybT, B_YBT, sgb, B_SGB, m2, B_M2)):
                    b = next_bank()
                    for k in range(8):
                        P.op("pe", lambda e, k=k, b=b, slot=slot, cn=cn, tc=tc: e.matmul(bank(b), lhsT=wbf[slot][:, k, cn * 128:(cn + 1) * 128],
                                                                                  rhs=hT[:, k, tc * 512:(tc + 1) * 512], start=(k == 0), stop=(k == 7)),
                             reads=[B_WB[slot], B_HT[tc]], writes=[PB[b]], sig=(k == 7))
                    P.op("act", lambda e, b=b, sg=sg: e.activation(out=sg, in_=bank(b), func=AF.Sigmoid), reads=[PB[b]], writes=[B_sg])
                    b2 = next_bank()
                    for c in range(4):
                        P.op("pe", lambda e, c=c, b2=b2, wp=wp, yT=yT, n=n, tc=tc: e.matmul(bank(b2), lhsT=wp[:, c, n * 128:(n + 1) * 128],
                                                                                      rhs=yT[:, c, tc * 512:(tc + 1) * 512], start=(c == 0), stop=(c == 3)),
                             reads=[B_WP, B_y], writes=[PB[b2]], sig=(c == 3))
                    P.op("dve", lambda e, b2=b2, sg=sg, mm=mm: e.tensor_tensor(out=mm, in0=sg, in1=bank(b2), op=ALU.mult), reads=[PB[b2], B_sg], writes=[B_mm])
                P.op("pool", lambda e, n=n, tc=tc: e.tensor_tensor(out=mT[:, n, tc * 512:(tc + 1) * 512], in0=m1, in1=m2, op=ALU.add),
                     reads=[B_M1, B_M2], writes=[B_MT])
    dump_fm("mT", mT, B_MT, 8)
    if stage <= 2.5:
        P.emit(finals)
        return nc, es

    P.barrier()
    A.top = md_mark
    xnT = hT
    B_XNT = B_HT
    wo = A.alloc([8, D], BF16)
    B_WO = Buf("wo")
    for kh in range(4):
        P.op("pool", lambda e, kh=kh: e.dma_start(out=wo[:, kh * 2:(kh + 1) * 2, :], in_=dap(wo_d, kh * 2 * 128 * D, [[D, 128], [128 * D, 2], [1, D]])),
             writes=[B_WO], dma=True, semkey=("c", 32))
    g2b = A.alloc([D], F32)
    P.op("sp", lambda e: e.dma_start(out=g2b, in_=dap(g2_d, 0, [[0, 128], [1, D]])), writes=[B_C], dma=True, semkey=("c", 33))
    xt2 = [A.alloc([D], F32) for _ in range(2)]
    x1t = [A.alloc([D], F32) for _ in range(2)]
    xnb = [A.alloc([D], BF16) for _ in range(2)]
    junk_ref[0] = A.alloc([D], BF16)
    B_XT2 = [Buf("xt2_0"), Buf("xt2_1")]
    B_X1T = [Buf("x1t0"), Buf("x1t1")]
    B_XNB = [Buf("xnb0"), Buf("xnb1")]
    B_X1S = [Buf("x1s%d" % i) for i in range(NT)]
    for i in range(NT):
        s_ = i % 2
        P.op("sp", lambda e, i=i, s_=s_: e.dma_start(out=xt2[s_], in_=x_d.ap()[i * 128:(i + 1) * 128, :]), writes=[B_XT2[s_]], dma=True, semkey=("xt2", s_))
        for half in range(2):
            b = next_bank()
            for k in range(8):
                P.op("pe", lambda e, k=k, b=b, i=i, half=half: e.matmul(bank(b), lhsT=mT[:, k, i * 128:(i + 1) * 128], rhs=wo[:, k, half * 512:(half + 1) * 512],
                                                                    start=(k == 0), stop=(k == 7)), reads=[B_MT, B_WO], writes=[PB[b]], sig=(k == 7))
            P.op("dve", lambda e, b=b, s_=s_, half=half: e.tensor_tensor(out=x1t[s_][:, half * 512:(half + 1) * 512], in0=xt2[s_][:, half * 512:(half + 1) * 512],
                                                                   in1=bank(b), op=ALU.add), reads=[PB[b], B_XT2[s_]], writes=[B_X1T[s_]])
        P.op("sp", lambda e, i=i, s_=s_: e.dma_start(out=x1s_d.ap()[i * 128:(i + 1) * 128, :], in_=x1t[s_]), reads=[B_X1T[s_]], writes=[B_X1S[i]],
             dma=True, semkey=("x1t", s_))
        rms_tile(i, x1t[s_], g2b, s_, [B_X1T[s_]], B_XNB[s_], xnb[s_])
        transpose_tile_to(i, xnb[s_], B_XNB[s_], xnT, B_XNT[i // 4], 6 + s_)
        if "x1" in dbg_d:
            dbg_out("x1", x1t[s_], [B_X1T[s_]], i * 128, 128, 0, D)
    if "xnT" in dbg_d:
        tmpf = A.alloc([1024], F32)
        B_T = Buf("tmpf_xn")
        for k in range(8):
            for hf in range(2):
                P.op("dve", lambda e, k=k, hf=hf: e.tensor_copy(out=tmpf, in_=xnT[:, k, hf * 1024:(hf + 1) * 1024]), reads=B_XNT, writes=[B_T])
                dbg_out("xnT", tmpf, [B_T], k * 128, 128, hf * 1024, 1024)
    if stage <= 3:
        P.emit(finals)
        return nc, es

    P.barrier()
    A.top = wbf_mark
    abgT = A.alloc([3, S], BF16)
    B_ABG = Buf("abgT")
    pk_mark = A.top
    A.top = att_mark
    wq = A.alloc([8, 2048], BF16)
    B_WQ = Buf("wq")
    for q4 in range(4):
        P.op("pool", lambda e, q4=q4: e.dma_start(out=wq[:, :, q4 * 512:(q4 + 1) * 512], in_=dap(wq_d, q4 * 512, [[2048, 128], [128 * 2048, 8], [1, 512]])),
             writes=[B_WQ], dma=True, semkey=("c", 40))
    skf = A.alloc([16, 128], F32)
    keysT = A.alloc([16, 128], BF16)
    B_SK, B_KT = Buf("skf"), Buf("keysT")
    P.op("sp", lambda e: e.dma_start(out=skf, in_=dap(sk_d, 0, [[128, 128], [128 * 128, 16], [1, 128]])), writes=[B_SK], dma=True, semkey=("c", 41))
    for hp in range(16):
        b = 4 + hp % 2
        P.op("pe", lambda e, hp=hp, b=b: e.transpose(bank(b, 0, 128), skf[:, hp, :], identf), reads=[B_SK, B_C], writes=[PB[b]])
        evac(keysT[:, hp, :], bank(b, 0, 128), [PB[b]], [B_KT])
    if stage <= 3.2:
        P.emit(finals)
        return nc, es
    qT = A.alloc([16, 512], BF16)
    B_QT = Buf("qT")
    sc = A.alloc([16, 128], F32)
    B_SC = Buf("sc")
    tv = A.alloc([16, 16], F32)
    tix = A.alloc([16, 16], U32)
    tixf = A.alloc([16, 16], F32)
    tmp1 = A.alloc([16, 128], F32)
    cand = A.alloc([8, 256], F32)
    tmp2 = A.alloc([8, 256], F32)
    bs = A.alloc([8, 16], F32)
    pos = A.alloc([8, 16], U32)
    k1u = A.alloc([128], U32)
    k2u = A.alloc([128], U32)
    k1f = A.alloc([128], F32)
    k2f = A.alloc([128], F32)
    eq1 = A.alloc([128, 16], F32)
    eq2 = A.alloc([128, 16], F32)
    abg = A.alloc([3, 128], F32)
    ee = A.alloc([8, 16], F32)
    zz = A.alloc([2, 8], F32)
    B_TK, B_PL, B_AB, B_EE = Buf("topk"), Buf("poolgather"), Buf("abg"), Buf("ee")
    B_T1 = [Buf("t1_%d" % i) for i in range(16)]
    B_T2 = [Buf("t2_%d" % i) for i in range(8)]

    ub = [A.alloc([D], BF16) for _ in range(2)]
    usb = [A.alloc([8, 128], BF16) for _ in range(2)]
    B_UB = [Buf("ub0"), Buf("ub1")]
    B_USB = [Buf("usb0"), Buf("usb1")]
    B_VS = [Buf("vs%d" % i) for i in range(32)]
    B_VSK = [Buf("vsk%d" % i) for i in range(4)]
    B_UTS = [Buf("uts%d" % i) for i in range(128)]

    def prepass_chunk(ec):
        if ec % 2 == 0:
            c2 = ec // 2
            P.op("pool", lambda e, c2=c2: e.dma_start(out=vs_d.ap()[c2 * 256:(c2 + 1) * 256, :], in_=v_d.ap()[c2 * 256:(c2 + 1) * 256, :]),
                 writes=[B_VS[c2 // 2], B_VSK[0]], dma=True, semkey=("vs", 0))
        s_ = ec % 2
        P.op("pool", lambda e, ec=ec, s_=s_: e.dma_start(out=ub[s_], in_=u_d.ap()[ec * 128:(ec + 1) * 128, :]), writes=[B_UB[s_]], dma=True, semkey=("ub", s_))
        b = 6 + s_
        pb = bankb(b)
        for k in range(8):
            P.op("pe", lambda e, k=k, s_=s_, pb=pb: e.transpose(pb[:, k * 128:(k + 1) * 128], ub[s_][:, k * 128:(k + 1) * 128], identb),
                 reads=[B_UB[s_], B_C], writes=[PB[b]], sig=(k == 7))
        evac(usb[s_].rearrange("p a b -> p (a b)"), pb, [PB[b]], [B_USB[s_]], eng="act")
        P.op("sp", lambda e, ec=ec, s_=s_: e.dma_start(out=dap(uts_d, ec * 131072, [[1024, 128], [1, 1024]]), in_=usb[s_].rearrange("p a b -> p (a b)")),
             reads=[B_USB[s_]], writes=[B_UTS[ec]], dma=True, semkey=("usb", s_))

    for tc in range(4):
        for hp in range(16):
            b = 4 + hp % 2
            for k in range(8):
                P.op("pe", lambda e, k=k, b=b, hp=hp, tc=tc: e.matmul(bank(b), lhsT=wq[:, k, hp * 128:(hp + 1) * 128], rhs=xnT[:, k, tc * 512:(tc + 1) * 512],
                                                                 start=(k == 0), stop=(k == 7)), reads=[B_WQ, B_XNT[tc]], writes=[PB[b]], sig=(k == 7))
            evac(qT[:, hp, :], bank(b), [PB[b]], [B_QT])
        for j in range(4):
            i = tc * 4 + j
            for hp in range(16):
                P.op("pe", lambda e, hp=hp, j=j: e.matmul(ps[:, hp * 128:(hp + 1) * 128], lhsT=qT[:, hp, j * 128:(j + 1) * 128], rhs=keysT[:, hp, :], start=True, stop=True),
                     reads=[B_QT, B_KT], writes=[PB[hp // 4]], sig=(hp == 15))
            P.op("act", lambda e: e.activation(out=sc.rearrange("p a b -> p (a b)"), in_=ps[:, 0:2048], func=AF.Copy), reads=PB[0:4], writes=[B_SC])
            if "sc" in dbg_d:
                dbg_out("sc", sc.rearrange("p a b -> p (a b)"), [B_SC], i * 128, 128, 0, 2048)
            if stage <= 3.4:
                P.emit(finals)
                return nc, es
            for cc in range(8):
                prepass_chunk(i * 8 + cc)
            for hp in range(16):
                P.op("dve", lambda e, hp=hp: e.max(out=tv[:, hp, 0:8], in_=sc[:, hp, :]), reads=[B_SC], writes=[B_T1[hp]], sig=(hp == 15))
            for hp in range(16):
                P.op("dve", lambda e, hp=hp: e.max_index(out=tix[:, hp, 0:8], in_max=tv[:, hp, 0:8], in_values=sc[:, hp, :]), reads=[B_SC, B_T1[hp]], writes=[B_T1[hp]], sig=(hp == 15))
            for hp in range(16):
                P.op("dve", lambda e, hp=hp: e.match_replace(out=tmp1[:, hp, :], in_to_replace=tv[:, hp, 0:8], in_values=sc[:, hp, :], imm_value=-1e30), reads=[B_SC, B_T1[hp]], writes=[B_T1[hp]], sig=(hp == 15))
            for hp in range(16):
                P.op("dve", lambda e, hp=hp: e.max(out=tv[:, hp, 8:16], in_=tmp1[:, hp, :]), reads=[B_T1[hp]], writes=[B_T1[hp]], sig=(hp == 15))
            for hp in range(16):
                P.op("dve", lambda e, hp=hp: e.max_index(out=tix[:, hp, 8:16], in_max=tv[:, hp, 8:16], in_values=tmp1[:, hp, :]), reads=[B_T1[hp]], writes=[B_T1[hp]], sig=(hp == 15))
            if stage <= 3.5:
                P.emit(finals)
                return nc, es
            P.op("pool", lambda e: e.tensor_copy(out=tixf.rearrange("p a b -> p (a b)"), in_=tix.rearrange("p a b -> p (a b)")), reads=B_T1, writes=[B_PL])
            if stage <= 3.6:
                P.emit(finals)
                return nc, es
            for h in range(8):
                P.op("dve", lambda e, h=h: e.tensor_tensor(out=cand[:, h, :].rearrange("p (a b) -> p a b", a=16),
                                                           in0=cap(tv, (2 * h) * 16, [[1, 16], [0, 16]]), in1=cap(tv, (2 * h + 1) * 16, [[0, 16], [1, 16]]), op=ALU.add),
                     reads=[B_T1[2 * h], B_T1[2 * h + 1]], writes=[B_T2[h]], sig=(h == 7))
            for h in range(8):
                P.op("dve", lambda e, h=h: e.max(out=bs[:, h, 0:8], in_=cand[:, h, :]), reads=[B_T2[h]], writes=[B_T2[h]], sig=(h == 7))
            for h in range(8):
                P.op("dve", lambda e, h=h: e.max_index(out=pos[:, h, 0:8], in_max=bs[:, h, 0:8], in_values=cand[:, h, :]), reads=[B_T2[h]], writes=[B_T2[h]], sig=(h == 7))
            for h in range(8):
                P.op("dve", lambda e, h=h: e.match_replace(out=tmp2[:, h, :], in_to_replace=bs[:, h, 0:8], in_values=cand[:, h, :], imm_value=-1e30), reads=[B_T2[h]], writes=[B_T2[h]], sig=(h == 7))
            for h in range(8):
                P.op("dve", lambda e, h=h: e.max(out=bs[:, h, 8:16], in_=tmp2[:, h, :]), reads=[B_T2[h]], writes=[B_T2[h]], sig=(h == 7))
            for h in range(8):
                P.op("dve", lambda e, h=h: e.max_index(out=pos[:, h, 8:16], in_max=bs[:, h, 8:16], in_values=tmp2[:, h, :]), reads=[B_T2[h]], writes=[B_T2[h]], sig=(h == 7))
            posf = pos.rearrange("p a b -> p (a b)")
            P.op("dve", lambda e: e.tensor_single_scalar(out=k1u, in_=posf, scalar=4, op=ALU.logical_shift_right), reads=B_T2, writes=[B_TK])
            P.op("dve", lambda e: e.tensor_single_scalar(out=k2u, in_=posf, scalar=15, op=ALU.bitwise_and), reads=B_T2 + [B_TK], writes=[B_TK])
            P.op("dve", lambda e: e.tensor_copy(out=k1f, in_=k1u), reads=[B_TK], writes=[B_TK])
            P.op("dve", lambda e: e.tensor_copy(out=k2f, in_=k2u), reads=[B_TK], writes=[B_TK])
            if stage <= 3.7:
                P.emit(finals)
                return nc, es
            for (kf, eq, pp, dst) in ((k1f, eq1, 0, 0), (k2f, eq2, 1, 1)):
                P.op("dve", lambda e, kf=kf, eq=eq: e.tensor_tensor(out=eq, in0=cap(iota16, 0, [[0, 128], [1, 16]]), in1=cap(kf, 0, [[1, 128], [0, 16]]), op=ALU.is_equal),
                     reads=[B_TK, B_C, B_AB], writes=[B_PL])
                for h in range(8):
                    P.op("pool", lambda e, eq=eq, h=h, pp=pp: e.tensor_tensor(out=eq[:, h * 16:(h + 1) * 16, :], in0=eq[:, h * 16:(h + 1) * 16, :],
                                                                        in1=cap(tixf, (2 * h + pp) * 16, [[0, 16], [1, 16]]), op=ALU.mult), reads=[B_PL], writes=[B_PL])
                P.op("dve", lambda e, eq=eq, dst=dst: e.tensor_reduce(out=abg[:, dst, :], in_=eq, axis=mybir.AxisListType.X, op=ALU.add), reads=[B_PL], writes=[B_AB])
            if stage <= 3.8:
                P.emit(finals)
                return nc, es
            P.op("dve", lambda e: e.tensor_tensor(out=ee, in0=bs, in1=cap(bs, 0, [[16, 8], [0, 16]]), op=ALU.subtract), reads=B_T2, writes=[B_EE])
            P.op("act", lambda e: e.activation(out=ee, in_=ee, func=AF.Exp), reads=[B_EE], writes=[B_EE])
            P.op("dve", lambda e: e.tensor_reduce(out=zz[:, 0, :], in_=ee, axis=mybir.AxisListType.X, op=ALU.add), reads=[B_EE], writes=[B_EE])
            P.op("dve", lambda e: e.reciprocal(out=zz[:, 1, :], in_=zz[:, 0, :]), reads=[B_EE], writes=[B_EE])
            P.op("dve", lambda e: e.tensor_tensor(out=abg[:, 2, :].rearrange("p (h j) -> p h j", h=8), in0=ee, in1=cap(zz, 8, [[1, 8], [0, 16]]), op=ALU.mult),
                 reads=[B_EE], writes=[B_AB])
            if "abg" in dbg_d:
                dbg_out("abg", abg.rearrange("p a b -> p (a b)"), [B_AB], i * 128, 128, 0, 384)
            if stage <= 3.9:
                P.emit(finals)
                return nc, es
            tb = 6 + i % 2
            for q in range(3):
                P.op("pe", lambda e, q=q, tb=tb: e.transpose(bank(tb, q * 128, (q + 1) * 128), abg[:, q, :], identf), reads=[B_AB, B_C], writes=[PB[tb]], sig=(q == 2))
            P.op("act", lambda e, tb=tb, i=i: e.activation(out=abgT[:, :, i * 128:(i + 1) * 128], in_=bank(tb, 0, 384).rearrange("p (q t) -> p q t", q=3), func=AF.Copy),
                 reads=[PB[tb]], writes=[B_ABG])
            if stage <= 3.95 and i + 1 >= PK_TILES:
                P.emit(finals)
                return nc, es
    if stage <= 4:
        P.emit(finals)
        return nc, es

    P.barrier()
    A.top = pk_mark
    GTh = [A.alloc([64, 256], BF16) for _ in range(2)]
    B_GTH = [Buf("GTlo"), Buf("GThi")]
    RAh = [A.alloc([16, 64], BF16) for _ in range(2)]
    LBe = [A.alloc([16, 128], BF16) for _ in range(2)]
    LB = [A.alloc([16, 128], BF16) for _ in range(2)]
    B_RA = [Buf("RA0"), Buf("RA1")]
    B_LBE = [Buf("LBe0"), Buf("LBe1")]
    B_LB = [Buf("LB0"), Buf("LB1")]
    NSB = 3
    uTb = [A.alloc([4, 8, 128], BF16) for _ in range(NSB)]
    vbuf = [A.alloc([4, D], BF16) for _ in range(NSB)]
    B_UTB = [Buf("utb%d" % i) for i in range(NSB)]
    B_VBUF = [Buf("vbuf%d" % i) for i in range(NSB)]
    B_GS = [Buf("gslot%d" % i) for i in range(4)]
    gl = [A.alloc([2, 256], BF16) for _ in range(2)]
    B_GL = [Buf("gl0"), Buf("gl1")]
    hidT = [A.alloc([256], BF16) for _ in range(4)]
    B_HID = [Buf("hid%d" % i) for i in range(4)]
    x2t = [A.alloc([D], F32) for _ in range(2)]
    B_X2 = [Buf("x2t0"), Buf("x2t1")]
    yt = A.alloc([D], F32)
    B_YT = Buf("yt")
    gfb = A.alloc([D], F32)
    junk_ref[0] = A.alloc([D], BF16)
    P.op("sp", lambda e: e.dma_start(out=gfb, in_=dap(gf_d, 0, [[0, 128], [1, D]])), writes=[B_C], dma=True, semkey=("c", 50))
    B_OUT = [Buf("out%d" % i) for i in range(NT)]
    NB = 8
    _gq = [0]
    _gbk = [0]

    def build_vec(X, half, s_):
        q_ = s_ % 2
        t0 = X * 256 + s_ * 16
        P.op("dve", lambda e, t0=t0, q_=q_, half=half: e.tensor_tensor(out=RAh[q_], in0=cap(iotab, half * 64, [[0, 16], [1, 64]]),
                                                                 in1=cap(abgT, 0 * S + t0, [[1, 16], [0, 64]]), op=ALU.is_equal),
             reads=[B_ABG, B_C], writes=[B_RA[q_]])
        P.op("dve", lambda e, t0=t0, q_=q_: e.tensor_tensor(out=LBe[q_], in0=cap(iotab, 0, [[0, 16], [1, 128]]), in1=cap(abgT, 1 * S + t0, [[1, 16], [0, 128]]), op=ALU.is_equal),
             reads=[B_ABG, B_C], writes=[B_LBE[q_]])
        P.op("pool", lambda e, t0=t0, q_=q_: e.tensor_tensor(out=LB[q_], in0=LBe[q_], in1=cap(abgT, 2 * S + t0, [[1, 16], [0, 128]]), op=ALU.mult),
             reads=[B_ABG, B_LBE[q_]], writes=[B_LB[q_]])

    def build_pe(X, half, s_):
        q_ = s_ % 2
        for t8 in range(2):
            gb = t8
            for tq in range(8):
                t = t8 * 8 + tq
                P.op("pe", lambda e, t=t, gb=gb, tq=tq, q_=q_: e.matmul(bank(gb, tq * 64, (tq + 1) * 64), lhsT=LB[q_][:, t, :], rhs=RAh[q_][:, t, :], start=True, stop=True),
                     reads=[B_LB[q_], B_RA[q_]], writes=[PB[gb]], sig=(tq == 7))
            tl = s_ * 16 + t8 * 8
            evac(cap(GTh[half], tl, [[256, 64], [1, 8]]), cap(psA, gb * 512, [[1, 64], [64, 8]]), [PB[gb]], [B_GTH[half]], eng="act")

    for s_ in range(16):
        build_vec(0, 0, s_)
        build_pe(0, 0, s_)

    for blk in range(NB):
        tb0 = blk * 256

        def emit_act(pr):
            ab = 2 + pr % 2
            for hf in range(2):
                i1 = pr * 2 + hf
                grp, ec = i1 // 4, i1 % 4
                s_ = (blk * 32 + grp) % NSB
                if ec == 0:
                    P.op("sp", lambda e, grp=grp, s_=s_: e.dma_start(out=uTb[s_].rearrange("p a k e -> p a (k e)"),
                                                                  in_=dap(uts_d, grp * 4 * 131072, [[1024, 128], [131072, 4], [1, 1024]])),
                         reads=B_UTS[grp * 4:(grp + 1) * 4], writes=[B_UTB[s_]], dma=True, semkey=("utb", s_))
                    P.op("sp", lambda e, grp=grp, s_=s_: e.dma_start(out=vbuf[s_], in_=dap(vs_d, grp * 512 * 1024, [[1024, 128], [128 * 1024, 4], [1, 1024]])),
                         reads=[B_VS[grp]], writes=[B_VBUF[s_]], dma=True, semkey=("vbuf", s_))
                for k in range(8):
                    P.op("pe", lambda e, k=k, ab=ab, hf=hf, ec=ec, s_=s_, tb0=tb0: e.matmul(bank(ab, hf * 256, (hf + 1) * 256), lhsT=uTb[s_][:, ec, k, :],
                                                                                      rhs=xnT[:, k, tb0:tb0 + 256], start=(k == 0), stop=(k == 7)),
                         reads=[B_UTB[s_], B_XNT[tb0 // 512]], writes=[PB[ab]], sig=(k == 7))

        def emit_out(pr):
            ab = 2 + pr % 2
            gs = pr % 2
            P.op("act", lambda e, ab=ab, gs=gs: e.activation(out=gl[gs].rearrange("p a b -> p (a b)"), in_=bank(ab), func=AF.Gelu), reads=[PB[ab]], writes=[B_GL[gs]])
            for hf in range(2):
                i1 = pr * 2 + hf
                grp, ec = i1 // 4, i1 % 4
                s_ = (blk * 32 + grp) % NSB
                hs = i1 % 4
                gh = i1 // 64
                P.op("dve", lambda e, gs=gs, hf=hf, i1=i1, hs=hs, gh=gh: e.tensor_tensor(out=hidT[hs], in0=gl[gs][:, hf, :], in1=GTh[gh][:, i1 % 64, :], op=ALU.mult),
                     reads=[B_GL[gs], B_GTH[gh]], writes=[B_HID[hs]])
                for tt in range(2):
                    for h2 in range(2):
                        ob = 4 + tt * 2 + h2
                        P.op("pe", lambda e, ob=ob, hs=hs, tt=tt, h2=h2, ec=ec, s_=s_, i1=i1: e.matmul(
                            bank(ob), lhsT=hidT[hs][:, tt * 128:(tt + 1) * 128], rhs=vbuf[s_][:, ec, h2 * 512:(h2 + 1) * 512],
                            start=(i1 == 0), stop=(i1 == 127)), reads=[B_HID[hs], B_VBUF[s_]], writes=[PB[ob]], sig=(tt == 1 and h2 == 1))

        emit_act(0)
        for pr in range(64):
            if pr + 1 < 64:
                emit_act(pr + 1)
            hph = pr // 32
            X, half = (blk, 1) if hph == 0 else (blk + 1, 0)
            j = pr % 32
            if X < NB:
                if j % 2 == 0:
                    if j >= 2:
                        build_pe(X, half, j // 2 - 1)
                    build_vec(X, half, j // 2)
                if j == 31:
                    build_pe(X, half, 15)
            emit_out(pr)

        for tt in range(2):
            i = blk * 2 + tt
            s_ = i % 2
            P.op("sp", lambda e, i=i, s_=s_: e.dma_start(out=x2t[s_], in_=x1s_d.ap()[i * 128:(i + 1) * 128, :]), reads=[B_X1S[i]], writes=[B_X2[s_]],
                 dma=True, semkey=("x2t", s_))
            for h2 in range(2):
                ob = 4 + tt * 2 + h2
                P.op("dve", lambda e, s_=s_, h2=h2, ob=ob: e.tensor_tensor(out=x2t[s_][:, h2 * 512:(h2 + 1) * 512], in0=x2t[s_][:, h2 * 512:(h2 + 1) * 512], in1=bank(ob), op=ALU.add),
                     reads=[PB[ob]], writes=[B_X2[s_]])
            if "x2" in dbg_d:
                dbg_out("x2", x2t[s_], [B_X2[s_]], i * 128, 128, 0, D)
            rms_tile(i, x2t[s_], gfb, s_, [B_X2[s_]], B_YT, yt)
            o = P.op("sp", lambda e, i=i: e.dma_start(out=out_d.ap()[i * 128:(i + 1) * 128, :], in_=yt), reads=[B_YT], writes=[B_OUT[i]], dma=True, semkey=("yt",))
            finals.append(o)

    P.emit(finals)
    return nc, es


def _host_consts(inputs):
    c = {}
    c["identb"] = np.eye(128, dtype=np.float32).astype(ml_dtypes.bfloat16)
    c["identf"] = np.eye(128, dtype=np.float32)
    c["iotab"] = np.tile(np.arange(128, dtype=np.float32)[None, :], (128, 1)).astype(ml_dtypes.bfloat16)
    c["iota16"] = np.tile(np.arange(16, dtype=np.float32)[None, :], (128, 1))
    rpb = np.asarray(inputs["na_rpb"], np.float32)[0]
    kc = np.arange(64)[:, None]
    cq = np.arange(64)[None, :]
    dci = np.clip(kc - cq, -15, 15) + 15
    g = rpb[:, :, dci]
    lo = g[:, 0:14].transpose(2, 1, 0, 3)
    hi = g[:, 1:15].transpose(2, 1, 0, 3)
    c["rbg"] = np.ascontiguousarray(np.concatenate([lo, hi], axis=0).reshape(128, 8 * 14 * 64), np.float32)
    cs = np.clip(np.arange(64) - 8, 0, 48)
    inwin = (kc >= cs[None, :]) & (kc < cs[None, :] + 16)
    nm = np.where(inwin, 0.0, NEG).astype(np.float32)
    c["negmask"] = np.ascontiguousarray(np.concatenate([nm, nm], axis=0))
    c["gq"] = np.ascontiguousarray(np.tile(np.asarray(inputs["gqa_q_norm_g"], np.float32)[0], 2).reshape(128, 1))
    c["gk"] = np.ascontiguousarray(np.tile(np.asarray(inputs["gqa_k_norm_g"], np.float32)[0], 2).reshape(128, 1))
    t = np.arange(S)
    inv = (10000.0 ** (-np.arange(16, dtype=np.float32) / 16)).astype(np.float32)
    ang_r = (t // 64).astype(np.float32)[None, :] * inv[:, None]
    ang_c = (t % 64).astype(np.float32)[None, :] * inv[:, None]
    ang = np.concatenate([ang_r, ang_r, ang_c, ang_c], axis=0)
    ang = np.concatenate([ang, ang], axis=0).astype(np.float32)
    c["rope_c"] = np.cos(ang).astype(np.float32)
    c["rope_s"] = np.sin(ang).astype(np.float32)
    R = np.zeros((128, 128), np.float32)
    for m in range(128):
        if m % 32 < 16:
            R[m, m + 16] = -1.0
        else:
            R[m, m - 16] = 1.0
    c["permT"] = np.ascontiguousarray(R.T)
    blk = np.zeros((128, 128), np.float32)
    blk[:64, :64] = 1.0 / 64
    blk[64:, 64:] = 1.0 / 64
    c["blk64"] = blk
    return c


def _in_maps(inputs, consts):
    shared = {
        "norm1_g": np.asarray(inputs["norm1_g"], np.float32).reshape(1, D),
        "w_in": np.asarray(inputs["w_in"], np.float32).reshape(D, 4352),
        "w_proj_a": np.asarray(inputs["w_proj_a"], np.float32).reshape(512, D),
        "w_proj_b": np.asarray(inputs["w_proj_b"], np.float32).reshape(512, D),
        "w_out": np.asarray(inputs["w_out"], np.float32).reshape(D, D),
        "norm2_g": np.asarray(inputs["norm2_g"], np.float32).reshape(1, D),
        "peer_w_q": np.asarray(inputs["peer_w_q"], np.float32).reshape(D, 2048),
        "peer_sub_keys": np.asarray(inputs["peer_sub_keys"], np.float32).reshape(16 * 128, 128),
        "peer_u": np.asarray(inputs["peer_u"], np.float32).reshape(16384, D),
        "peer_v": np.asarray(inputs["peer_v"], np.float32).reshape(16384, D),
        "norm_f_g": np.asarray(inputs["norm_f_g"], np.float32).reshape(1, D),
    }
    shared.update(consts)
    x = np.asarray(inputs["x"], np.float32)
    return [dict(shared, x=np.ascontiguousarray(x[b])) for b in range(8)]


def kernel(**inputs):
    nc, es = build()
    with es:
        pass
    maps = _in_maps(inputs, _host_consts(inputs))
    res = run_bass_kernel_spmd(nc, maps, core_ids=list(range(8)))
    return np.stack([r["out"] for r in res.results], axis=0).astype(np.float32)
```
